# Optimizing a Trainium2 kernel written in Bass

```python
import math
import jax, jax.numpy as jnp
from jax import lax
import numpy as np

D_MODEL = 1024
BATCH = 8
SEQ = 4096
DEPTH = 2

CONV_WIDTH = 512
CONV_A_K = 3
N_HEADS = 8
HEAD_DIM = 64
ATTN_WIDTH = N_HEADS * HEAD_DIM
ROT_DIM = HEAD_DIM // 4
ROPE_THETA = 500000.0
MOBA_BLOCK = 256
MOBA_TOPK = 3
Q_CHUNK = 64
LRU_WIDTH = 512
LRU_BLOCKS = 8
LRU_BLOCK_DIM = LRU_WIDTH // LRU_BLOCKS
CONV_C_K = 4
LRU_C = 8.0
N_BRANCHES = 3
IN_COLS = 3 * CONV_WIDTH + 3 * ATTN_WIDTH + 2 * LRU_WIDTH + N_BRANCHES * D_MODEL
D_FF = 2816
N_EXPERTS = 8
TOP_K = 2
EXPERT_FF = 3584
EPS = 1e-6
NEG = -1e30

kernel_name = "hybrid_gated_conv_moba_rglru_moe"


def rms_norm(x, g):
    xf = x.astype(jnp.float32)
    y = xf * lax.rsqrt(jnp.mean(xf * xf, axis=-1, keepdims=True) + EPS)
    return (y * g.astype(jnp.float32)).astype(x.dtype)


def causal_depthwise_conv(u, w):
    k_w, c = w.shape
    return lax.conv_general_dilated(
        u, w[:, None, :].astype(u.dtype), window_strides=(1,), padding=[(k_w - 1, 0)],
        dimension_numbers=("NWC", "WIO", "NWC"), feature_group_count=c)


def partial_rotary(t, pos):
    half = ROT_DIM // 2
    inv_freq = ROPE_THETA ** (-jnp.arange(0, ROT_DIM, 2, dtype=jnp.float32) / ROT_DIM)
    ang = pos.astype(jnp.float32)[:, None] * inv_freq[None, :]
    cos = jnp.cos(ang)[None, :, None, :]
    sin = jnp.sin(ang)[None, :, None, :]
    r1 = t[..., :half].astype(jnp.float32)
    r2 = t[..., half:ROT_DIM].astype(jnp.float32)
    rot = jnp.concatenate([r1 * cos - r2 * sin, r2 * cos + r1 * sin], axis=-1).astype(t.dtype)
    return jnp.concatenate([rot, t[..., ROT_DIM:]], axis=-1)


def moba_attention(q, k, v):
    b, s, h, dh = q.shape
    s_pad = -(-s // MOBA_BLOCK) * MOBA_BLOCK
    pad = ((0, 0), (0, s_pad - s), (0, 0), (0, 0))
    q, k, v = [jnp.pad(t, pad).transpose(0, 2, 1, 3) for t in (q, k, v)]
    nb = s_pad // MOBA_BLOCK
    n_sel = min(MOBA_TOPK, nb)
    kb = k.reshape(b, h, nb, MOBA_BLOCK, dh)
    vb = v.reshape(b, h, nb, MOBA_BLOCK, dh)
    k_mean = jnp.mean(kb.astype(jnp.float32), axis=3)
    n_chunks = s_pad // Q_CHUNK
    chunks_per_block = MOBA_BLOCK // Q_CHUNK
    q_chunks = q.reshape(b, h, n_chunks, Q_CHUNK, dh).transpose(2, 0, 1, 3, 4)
    b_idx = jnp.arange(b)[:, None, None, None]
    h_idx = jnp.arange(h)[None, :, None, None]
    scale = dh ** -0.5

    def chunk(args):
        q_c, c = args
        blk = c // chunks_per_block
        q_pos = c * Q_CHUNK + jnp.arange(Q_CHUNK)
        gate = jnp.einsum("bhqd,bhnd->bhqn", q_c.astype(jnp.float32), k_mean)
        gate = jnp.where(jnp.arange(nb) < blk, gate, NEG)
        _, sel = lax.top_k(gate, n_sel)
        k_sel = kb[b_idx, h_idx, sel]
        v_sel = vb[b_idx, h_idx, sel]
        s_sel = jnp.einsum("bhqd,bhqkjd->bhqkj", q_c, k_sel).astype(jnp.float32) * scale
        s_sel = jnp.where((jnp.arange(n_sel) < blk)[:, None], s_sel, NEG)
        k_own = lax.dynamic_index_in_dim(kb, blk, axis=2, keepdims=False)
        v_own = lax.dynamic_index_in_dim(vb, blk, axis=2, keepdims=False)
        k_pos = blk * MOBA_BLOCK + jnp.arange(MOBA_BLOCK)
        s_own = jnp.einsum("bhqd,bhjd->bhqj", q_c, k_own).astype(jnp.float32) * scale
        s_own = jnp.where(k_pos[None, :] <= q_pos[:, None], s_own, NEG)
        scores = jnp.concatenate([s_sel.reshape(b, h, Q_CHUNK, n_sel * MOBA_BLOCK), s_own], axis=-1)
        p = jax.nn.softmax(scores, axis=-1).astype(v.dtype)
        p_sel = p[..., :n_sel * MOBA_BLOCK].reshape(b, h, Q_CHUNK, n_sel, MOBA_BLOCK)
        p_own = p[..., n_sel * MOBA_BLOCK:]
        return (jnp.einsum("bhqkj,bhqkjd->bhqd", p_sel, v_sel)
                + jnp.einsum("bhqj,bhjd->bhqd", p_own, v_own))

    out = lax.map(chunk, (q_chunks, jnp.arange(n_chunks)))
    out = out.transpose(1, 0, 3, 2, 4).reshape(b, s_pad, h * dh)
    return out[:, :s]


def rg_lru(xc, w_r, b_r, w_i, b_i, lam):
    bsz, s, _ = xc.shape
    xb = xc.reshape(bsz, s, LRU_BLOCKS, LRU_BLOCK_DIM)
    r = jax.nn.sigmoid((jnp.einsum("bshi,hij->bshj", xb, w_r).reshape(bsz, s, LRU_WIDTH) + b_r).astype(jnp.float32))
    i = jax.nn.sigmoid((jnp.einsum("bshi,hij->bshj", xb, w_i).reshape(bsz, s, LRU_WIDTH) + b_i).astype(jnp.float32))
    log_a = -LRU_C * r * jax.nn.softplus(-lam.astype(jnp.float32))
    a = jnp.exp(log_a)
    u = jnp.sqrt(-jnp.expm1(2.0 * log_a)) * (i * xc.astype(jnp.float32))

    def combine(c1, c2):
        a1, b1 = c1
        a2, b2 = c2
        return a1 * a2, a2 * b1 + b2

    _, hs = lax.associative_scan(combine, (a, u), axis=1)
    return hs.astype(xc.dtype)


def swiglu(h, w_gate, w_up, w_down):
    return (jax.nn.silu(h @ w_gate) * (h @ w_up)) @ w_down


def moe_swiglu(h, router, w_gate, w_up, w_down):
    bsz, s, d = h.shape
    t = h.reshape(-1, d)
    logits = (t @ router).astype(jnp.float32)
    top_v, top_i = lax.top_k(logits, TOP_K)
    w = jax.nn.softmax(top_v, axis=-1)
    comb = jnp.sum(jax.nn.one_hot(top_i, N_EXPERTS, dtype=jnp.float32) * w[..., None], axis=1)

    def expert_step(acc, params):
        wg, wu, wd, c_e = params
        return acc + c_e[:, None].astype(t.dtype) * swiglu(t, wg, wu, wd), None

    out, _ = lax.scan(expert_step, jnp.zeros_like(t), (w_gate, w_up, w_down, comb.T))
    return out.reshape(bsz, s, d)


def setup_inputs(seed: int = 0) -> dict:
    key = jax.random.key(seed)
    ks = iter(jax.random.split(key, 32))
    L = DEPTH
    n_dense = (DEPTH + 1) // 2
    n_moe = DEPTH // 2

    def nrm(shape, fan_in):
        return jax.random.normal(next(ks), shape, jnp.float32) * (fan_in ** -0.5)

    def gain(shape):
        return 1.0 + 0.02 * jax.random.normal(next(ks), shape, jnp.float32)

    def bias(shape):
        return 0.02 * jax.random.normal(next(ks), shape, jnp.float32)

    a0 = jax.random.uniform(next(ks), (L, LRU_WIDTH), jnp.float32, 0.9, 0.999)
    return {
        "x": jax.random.normal(next(ks), (BATCH, SEQ, D_MODEL), jnp.float32),
        "norm_mix": gain((L, D_MODEL)),
        "w_in": nrm((L, D_MODEL, IN_COLS), D_MODEL),
        "conv_a_w": nrm((L, CONV_A_K, CONV_WIDTH), CONV_A_K),
        "wo_a": nrm((L, CONV_WIDTH, D_MODEL), CONV_WIDTH),
        "q_norm": gain((L, HEAD_DIM)),
        "k_norm": gain((L, HEAD_DIM)),
        "wo_b": nrm((L, ATTN_WIDTH, D_MODEL), ATTN_WIDTH),
        "conv_c_w": nrm((L, CONV_C_K, LRU_WIDTH), CONV_C_K),
        "conv_c_b": bias((L, LRU_WIDTH)),
        "rg_w_r": nrm((L, LRU_BLOCKS, LRU_BLOCK_DIM, LRU_BLOCK_DIM), LRU_BLOCK_DIM),
        "rg_b_r": bias((L, LRU_WIDTH)),
        "rg_w_i": nrm((L, LRU_BLOCKS, LRU_BLOCK_DIM, LRU_BLOCK_DIM), LRU_BLOCK_DIM),
        "rg_b_i": bias((L, LRU_WIDTH)),
        "rg_lambda": jnp.log(a0 / (1.0 - a0)),
        "wo_c": nrm((L, LRU_WIDTH, D_MODEL), LRU_WIDTH),
        "w_o": nrm((L, D_MODEL, D_MODEL), D_MODEL),
        "norm_ffn": gain((L, D_MODEL)),
        "ffn_w_gate": nrm((n_dense, D_MODEL, D_FF), D_MODEL),
        "ffn_w_up": nrm((n_dense, D_MODEL, D_FF), D_MODEL),
        "ffn_w_down": nrm((n_dense, D_FF, D_MODEL), D_FF),
        "moe_router": nrm((n_moe, D_MODEL, N_EXPERTS), D_MODEL),
        "moe_w_gate": nrm((n_moe, N_EXPERTS, D_MODEL, EXPERT_FF), D_MODEL),
        "moe_w_up": nrm((n_moe, N_EXPERTS, D_MODEL, EXPERT_FF), D_MODEL),
        "moe_w_down": nrm((n_moe, N_EXPERTS, EXPERT_FF, D_MODEL), EXPERT_FF),
    }


def reference(x, norm_mix, w_in, conv_a_w, wo_a, q_norm, k_norm, wo_b, conv_c_w, conv_c_b,
              rg_w_r, rg_b_r, rg_w_i, rg_b_i, rg_lambda, wo_c, w_o, norm_ffn,
              ffn_w_gate, ffn_w_up, ffn_w_down, moe_router, moe_w_gate, moe_w_up, moe_w_down):
    bsz, s, d = x.shape
    pos = jnp.arange(s, dtype=jnp.int32)
    sizes = [CONV_WIDTH] * 3 + [ATTN_WIDTH] * 3 + [LRU_WIDTH] * 2 + [N_BRANCHES * D_MODEL]
    cuts = list(np.cumsum(sizes)[:-1])
    for l in range(DEPTH):
        h = rms_norm(x, norm_mix[l])
        p = h @ w_in[l]
        xa, ba, ca, q, k, v, xc, gc, gates = jnp.split(p, cuts, axis=-1)
        y_a = (ba * causal_depthwise_conv(ca * xa, conv_a_w[l])) @ wo_a[l]
        q = partial_rotary(rms_norm(q.reshape(bsz, s, N_HEADS, HEAD_DIM), q_norm[l]), pos)
        k = partial_rotary(rms_norm(k.reshape(bsz, s, N_HEADS, HEAD_DIM), k_norm[l]), pos)
        v = v.reshape(bsz, s, N_HEADS, HEAD_DIM)
        y_b = moba_attention(q, k, v) @ wo_b[l]
        xc = causal_depthwise_conv(xc, conv_c_w[l]) + conv_c_b[l]
        hc = rg_lru(xc, rg_w_r[l], rg_b_r[l], rg_w_i[l], rg_b_i[l], rg_lambda[l])
        y_c = (jax.nn.gelu(gc, approximate=True) * hc) @ wo_c[l]
        g = jax.nn.sigmoid(gates.reshape(bsz, s, N_BRANCHES, d))
        merged = g[:, :, 0] * y_a + g[:, :, 1] * y_b + g[:, :, 2] * y_c
        x = x + merged @ w_o[l]
        h = rms_norm(x, norm_ffn[l])
        if l % 2 == 0:
            x = x + swiglu(h, ffn_w_gate[l // 2], ffn_w_up[l // 2], ffn_w_down[l // 2])
        else:
            x = x + moe_swiglu(h, moe_router[l // 2], moe_w_gate[l // 2], moe_w_up[l // 2], moe_w_down[l // 2])
    return x
```

```python
import os
import numpy as np
from contextlib import ExitStack
import concourse.bass as bass
import concourse.mybir as mybir
from concourse.bass_utils import run_bass_kernel_spmd

F32 = mybir.dt.float32
BF16 = mybir.dt.bfloat16
AF = mybir.ActivationFunctionType
ALU = mybir.AluOpType
AX = mybir.AxisListType

SEM_LIMIT = 30000
EPS = 1e-6
LRU_C = 8.0
MOBA_TOPK = 3
ROT_DIM = 16
ROPE_THETA = 500000.0


class Sched:
    ENGS = ("pe", "act", "dve", "pool", "sp")

    def __init__(self, nc):
        self.nc = nc
        self.ops = []

    def add(self, eng, fn, reads=(), writes=(), dma=None):
        assert eng in self.ENGS
        self.ops.append(dict(eng=eng, fn=fn, reads=tuple(reads), writes=tuple(writes), dma=dma,
                             deps=set(), signal=False))
        return len(self.ops) - 1

    def analyze(self):
        last_w = {}
        readers = {}
        for i, op in enumerate(self.ops):
            deps = set()
            for r in op["reads"]:
                if r in last_w:
                    deps.add(last_w[r])
            for w in op["writes"]:
                if w in last_w:
                    deps.add(last_w[w])
                for rd in readers.get(w, ()):
                    deps.add(rd)
            deps.discard(i)
            op["deps"] = deps
            for r in op["reads"]:
                readers.setdefault(r, []).append(i)
            for w in op["writes"]:
                last_w[w] = i
                readers[w] = []
        for i, op in enumerate(self.ops):
            if op["eng"] == "pe" and op["dma"] is None:
                op["deps"] = {d for d in op["deps"]
                              if not (self.ops[d]["eng"] == "pe" and self.ops[d]["dma"] is None)}
        for op in self.ops:
            if op["dma"] is not None:
                op["signal"] = True
            for d in op["deps"]:
                self.ops[d]["signal"] = True

    def emit(self, stack):
        nc = self.nc
        self.analyze()
        eng_sem = {}
        eng_cnt = {}
        sems = []

        def new_sem(name):
            s = stack.enter_context(nc.semaphore(name))
            sems.append(s)
            return s

        dma_sem = {}
        dma_cnt = {}
        for e in self.ENGS:
            eng_sem[e] = new_sem(f"s_{e}_0")
            eng_cnt[e] = 0
        for i, op in enumerate(self.ops):
            if not op["signal"]:
                op["sig"] = None
                continue
            if op["dma"] is not None:
                k = op["dma"]
                if k not in dma_sem or dma_cnt[k] + 16 > SEM_LIMIT:
                    dma_sem[k] = new_sem(f"d{len(sems)}")
                    dma_cnt[k] = 0
                dma_cnt[k] += 16
                op["sig"] = (dma_sem[k], dma_cnt[k], 16)
            else:
                e = op["eng"]
                if eng_cnt[e] + 1 > SEM_LIMIT:
                    eng_sem[e] = new_sem(f"s_{e}_{i}")
                    eng_cnt[e] = 0
                eng_cnt[e] += 1
                op["sig"] = (eng_sem[e], eng_cnt[e], 1)
        self.n_sems = len(sems)
        block = stack.enter_context(nc.Block())
        ops = self.ops

        def make(engname):
            def body(eng):
                known = {}
                for op in ops:
                    if op["eng"] != engname:
                        continue
                    for d in sorted(op["deps"]):
                        sem, val, _ = ops[d]["sig"]
                        key = id(sem)
                        if known.get(key, 0) >= val:
                            continue
                        eng.wait_ge(sem, val)
                        known[key] = val
                    ins = op["fn"](eng)
                    if op["sig"] is not None:
                        sem, val, inc = op["sig"]
                        ins.then_inc(sem, inc)
            return body

        block.tensor(make("pe"))
        block.scalar(make("act"))
        block.vector(make("dve"))
        block.gpsimd(make("pool"))
        block.sync(make("sp"))


class RPool:
    def __init__(self, name, tiles):
        self.name = name
        self.tiles = tiles
        self.i = 0

    def get(self):
        k = self.i % len(self.tiles)
        self.i += 1
        return self.tiles[k], f"{self.name}{k}"


PP_GMIX, PP_GFFN, PP_CWA, PP_CWC, PP_CBC, PP_BR, PP_BI, PP_LAM = 0, 8, 16, 28, 44, 48, 52, 56
NEGM = -30000.0
NEGB = -240000.0


def build_nc(S=4096, L=2, dbg=False, upto=99):
    nc = bass.Bass("TRN2", target_bir_lowering=False)
    NCH = S // 512
    NT = S // 128
    NB = S // 256
    D = 1024

    def din(name, shape, dt=F32):
        return nc.dram_tensor(name, shape, dt, kind="ExternalInput").ap()

    x = din("x", [S, D])
    w_in = din("w_in", [2 * 1024, 7168])
    wo_abc = [din("wo_a", [2 * 512, 1024]), din("wo_b", [2 * 512, 1024]), din("wo_c", [2 * 512, 1024])]
    w_o = din("w_o", [2 * 1024, 1024])
    ffn_g = din("ffn_w_gate", [1024, 2816])
    ffn_u = din("ffn_w_up", [1024, 2816])
    ffn_d = din("ffn_w_down", [2816, 1024])
    moe_r = din("moe_router", [1024, 8])
    moe_g = din("moe_w_gate", [8 * 1024, 3584])
    moe_u = din("moe_w_up", [8 * 1024, 3584])
    moe_d = din("moe_w_down", [8 * 3584, 1024])
    rg_r = din("rg_w_r", [2 * 8 * 64, 64])
    rg_i = din("rg_w_i", [2 * 8 * 64, 64])
    pp = din("pp", [2 * 128, 64])
    gqk = din("gqk", [2 * 128, 1024])
    rope = din("rope", [S, 128])
    y = nc.dram_tensor("y", [S, D], F32, kind="ExternalOutput").ap()
    xmid = nc.dram_tensor("xmid", [S, D], F32, kind="Internal").ap()
    dbg_out = {}
    if dbg:
        for nm, shp in (("d_za", [512, S]), ("d_zb", [512, S]), ("d_zc", [512, S]), ("d_mg", [1024, S]),
                        ("d_xmix", [S, 1024])):
            dbg_out[nm] = nc.dram_tensor(nm, shp, F32 if nm == "d_xmix" else BF16, kind="ExternalOutput").ap()

    with ExitStack() as st:
        def sb(name, shape, dt=F32):
            return st.enter_context(nc.sbuf_tensor(name, shape, dt))

        def psum(name, shape, dt=F32):
            return st.enter_context(nc.psum_tensor(name, shape, dt))

        Sc = Sched(nc)

        stage = [0]

        def A(eng, fn, r=(), w=(), dma=None):
            if stage[0] <= upto:
                Sc.add(eng, fn, reads=r, writes=w, dma=dma)

        qn_bf = sb("qn_bf", [128, 512], BF16)
        id_f = sb("id_f", [128, 128])
        id_bf = sb("id_bf", [128, 128], BF16)
        triT = sb("triT", [128, 128])
        ones_bf = sb("ones_bf", [128, 128], BF16)
        ppt = sb("ppt", [128, 64])
        pp2 = ppt[:]
        gqk_sb = sb("gqk_sb", [128, 1024])
        bd_r = sb("bd_r", [128, 4, 128], BF16)
        bd_i = sb("bd_i", [128, 4, 128], BF16)
        R_sb = sb("R_sb", [128, 8, 8])
        lc = sb("lc", [128, 12])
        xt = sb("xt", [128, 4, 1024])
        junk = sb("junk", [128, 1024], BF16)
        ss = sb("ss", [128, 8])
        xs_pool = RPool("xs", [sb(f"xs{i}", [128, 1024]) for i in range(2)])
        h2f = sb("h2f", [128, 8, 128])
        hT = sb("hT", [128, 8, 512], BF16)
        ws = RPool("ws", [sb(f"ws{i}", [128, 4096], BF16) for i in range(3)])
        zT = [sb(f"zT{i}", [128, 4, 512], BF16) for i in range(3)]
        macc = sb("macc", [128, 4, 512])
        mergedT = sb("mergedT", [128, 8, 512], BF16)
        aT_pool = RPool("aT", [sb(f"aT{i}", [128, 4, 512], BF16) for i in range(2)])
        kT = sb("kT", [128, 4, S], BF16)
        v_all = sb("v_all", [128, NT, 512], BF16)
        qT = sb("qT", [128, 8, 512], BF16)
        cs = sb("cs", [128, 4, 128])
        histA = sb("histA", [128, 4, 2])
        histC = sb("histC", [128, 4, 3])
        hst = sb("hst", [128, 4])
        ext = sb("ext", [128, 515])
        wk = RPool("wk", [sb(f"wk{i}", [128, 512]) for i in range(6)])
        wkb = RPool("wkb", [sb(f"wkb{i}", [128, 512], BF16) for i in range(2)])
        sm = sb("sm", [128, 64])
        kmean_f = sb("kmean_f", [128, 4, 16])
        kmean_bf = sb("kmean_bf", [128, 4, 16], BF16)
        gate_sb = sb("gate_sb", [128, 8, 16])
        m8 = sb("m8", [128, 64])
        bias_sb = sb("bias_sb", [128, 8, 16])
        PT_pool = RPool("PT", [sb(f"PT{i}", [128, 256], BF16) for i in range(4)])
        B2_pool = RPool("B2", [sb(f"B2{i}", [128, 128], BF16) for i in range(2)])
        dtmp = RPool("dtmp", [sb(f"dtmp{i}", [128, 128]) for i in range(2)])
        comb = sb("comb", [128, 4, 8])
        lg = sb("lg", [128, 8])
        mm = RPool("mm", [psum(f"mm{i}", [128, 512]) for i in range(3)])
        tp = RPool("tp", [psum(f"tp{i}", [128, 1024], BF16) for i in range(1)])
        psOut = [psum("psOa", [128, 512]), psum("psOb", [128, 512])]
        psDen = [psum("psDa", [128, 512]), psum("psDb", [128, 512])]
        accO = [sb(f"accO{i}", [128, 128]) for i in range(2)]
        accD = [sb(f"accD{i}", [128, 128]) for i in range(2)]
        pvc = [0]

        def col(j):
            return pp2[:, j:j + 1]

        A("pool", lambda e: e.memset(id_f[:], 1.0), w=["id_f"])
        A("pool", lambda e: e.affine_select(out=id_f[:], in_=id_f[:], pattern=[[-1, 128]], compare_op=ALU.is_equal,
                                            fill=0.0, base=0, channel_multiplier=1), r=["id_f"], w=["id_f"])
        A("dve", lambda e: e.tensor_copy(out=id_bf[:], in_=id_f[:]), r=["id_f"], w=["id_bf"])
        A("pool", lambda e: e.memset(triT[:], 0.0), w=["triT"])
        A("pool", lambda e: e.affine_select(out=triT[:], in_=triT[:], pattern=[[1, 128]], compare_op=ALU.is_ge,
                                            fill=NEGM, base=0, channel_multiplier=-1), r=["triT"], w=["triT"])
        A("dve", lambda e: e.memset(ones_bf[:], 1.0), w=["ones_bf"])
        A("dve", lambda e: e.memset(qT[:], 0.0), w=["qT0", "qT1", "qT2", "qT3"])
        A("sp", lambda e: e.dma_start(out=R_sb[:], in_=moe_r.rearrange("(k p) e -> p k e", p=128)), w=["R_sb"], dma="R_sb")

        def load_slab(src_ap, shape3):
            t, nm = ws.get()
            a, b = shape3
            v = t[:, 0:a * b].rearrange("p (a b) -> p a b", a=a)
            A("pool", lambda e: e.dma_start(out=v, in_=src_ap), w=[nm], dma=nm)
            return v, nm

        def mm_group(ps_ap, pairs, r, w):
            n = len(pairs)

            def fn(e):
                ins = None
                for i, (lt, rh) in enumerate(pairs):
                    ins = e.matmul(ps_ap, lhsT=lt, rhs=rh, start=(i == 0), stop=(i == n - 1))
                return ins
            A("pe", fn, r=r, w=w)

        def rmsnorm(g0, router):
            for s in range(4):
                A("act", lambda e, s=s: e.activation(out=junk[:], in_=xt[:, s, :], func=AF.Square,
                                                     accum_out=ss[:, s:s + 1]), r=["xt"], w=["junk", "ss"])
            A("act", lambda e: e.activation(out=ss[:, 4:8], in_=ss[:, 0:4], func=AF.Sqrt, scale=1.0 / D, bias=EPS),
              r=["ss"], w=["ss"])
            A("dve", lambda e: e.reciprocal(out=ss[:, 4:8], in_=ss[:, 4:8]), r=["ss"], w=["ss"])
            for s in range(4):
                xs, xsn = xs_pool.get()
                A("dve", lambda e, s=s, xs=xs: e.tensor_scalar(out=xs[:], in0=xt[:, s, :], scalar1=ss[:, 4 + s:5 + s],
                                                              scalar2=None, op0=ALU.mult), r=["xt", "ss"], w=[xsn])
                for hf in range(2):
                    bank, bn = mm.get()

                    def fn(e, xs=xs, bank=bank, hf=hf):
                        ins = None
                        for kk in range(4):
                            k = hf * 4 + kk
                            ins = e.transpose(out=bank[:, kk * 128:(kk + 1) * 128], in_=xs[:, k * 128:(k + 1) * 128],
                                              identity=id_f[:])
                        return ins
                    A("pe", fn, r=[xsn, "id_f"], w=[bn])
                    bv = bank[:].rearrange("p (k t) -> p k t", k=4)
                    gb = pp2[:, g0 + hf * 4:g0 + hf * 4 + 4].rearrange("p (k o) -> p k o", o=1).to_broadcast([128, 4, 128])
                    if router:
                        A("dve", lambda e, bv=bv, gb=gb, hf=hf: e.tensor_tensor(out=h2f[:, hf * 4:hf * 4 + 4, :], in0=bv, in1=gb,
                                                                              op=ALU.mult), r=[bn, "ppt"], w=[f"h2f{hf}"])
                        A("act", lambda e, hf=hf, s=s: e.copy(out=hT[:, hf * 4:hf * 4 + 4, s * 128:(s + 1) * 128],
                                                             in_=h2f[:, hf * 4:hf * 4 + 4, :]), r=[f"h2f{hf}"], w=[f"hT{s}"])
                    else:
                        A("dve", lambda e, bv=bv, gb=gb, hf=hf, s=s: e.tensor_tensor(
                            out=hT[:, hf * 4:hf * 4 + 4, s * 128:(s + 1) * 128], in0=bv, in1=gb, op=ALU.mult),
                          r=[bn, "ppt"], w=[f"hT{s}"])
                if router:
                    pl_, pln = mm.get()
                    mm_group(pl_[:, 0:8], [(h2f[:, k, :], R_sb[:, k, :]) for k in range(8)], r=["h2f0", "h2f1", "R_sb"], w=[pln])
                    A("dve", lambda e, pl_=pl_: e.tensor_copy(out=lg[:], in_=pl_[:, 0:8]), r=[pln], w=["lg"])
                    A("dve", lambda e: e.max(out=m8[:, 0:8], in_=lg[:]), r=["lg"], w=["m8"])
                    A("dve", lambda e: e.tensor_tensor(out=sm[:, 0:1], in0=m8[:, 1:2], in1=m8[:, 0:1], op=ALU.subtract), r=["m8"], w=["sm"])
                    A("act", lambda e: e.activation(out=sm[:, 1:2], in_=sm[:, 0:1], func=AF.Exp), r=["sm"], w=["sm"])
                    A("dve", lambda e: e.tensor_scalar(out=sm[:, 2:3], in0=sm[:, 1:2], scalar1=1.0, scalar2=None, op0=ALU.add), r=["sm"], w=["sm"])
                    A("dve", lambda e: e.reciprocal(out=sm[:, 2:3], in_=sm[:, 2:3]), r=["sm"], w=["sm"])
                    A("dve", lambda e: e.tensor_tensor(out=sm[:, 3:4], in0=sm[:, 1:2], in1=sm[:, 2:3], op=ALU.mult), r=["sm"], w=["sm"])
                    A("dve", lambda e, s=s: e.tensor_scalar(out=comb[:, s, :], in0=lg[:], scalar1=m8[:, 0:1], scalar2=sm[:, 2:3],
                                                           op0=ALU.is_equal, op1=ALU.mult), r=["lg", "m8", "sm"], w=["comb"])
                    A("dve", lambda e: e.tensor_scalar(out=sm[:, 8:16], in0=lg[:], scalar1=m8[:, 1:2], scalar2=sm[:, 3:4],
                                                      op0=ALU.is_equal, op1=ALU.mult), r=["lg", "m8", "sm"], w=["sm"])
                    A("dve", lambda e, s=s: e.tensor_tensor(out=comb[:, s, :], in0=comb[:, s, :], in1=sm[:, 8:16], op=ALU.add),
                      r=["comb", "sm"], w=["comb"])
            return [f"hT{s}" for s in range(4)]

        HT = [f"hT{s}" for s in range(4)]
        KTN = lambda t: f"kT{t}"
        VN = lambda t: f"v{t}"

        for l in range(L):
            src = x if l == 0 else xmid
            dst = y if l == L - 1 else xmid
            dstn = "y" if l == L - 1 else "xmid"
            srcn = "x" if l == 0 else "xmid"
            A("sp", lambda e, l=l: e.dma_start(out=ppt[:], in_=pp[l * 128:(l + 1) * 128, :]), w=["ppt"], dma="ppt")
            A("sp", lambda e, l=l: e.dma_start(out=gqk_sb[:], in_=gqk[l * 128:(l + 1) * 128, :]), w=["gqk"], dma="gqk")
            for (bd, src_w, nm) in ((bd_r, rg_r, "bd_r"), (bd_i, rg_i, "bd_i")):
                A("dve", lambda e, bd=bd: e.memset(bd[:], 0.0), w=[nm])
                for h in range(8):
                    hp = (h % 2) * 64
                    r0 = (l * 8 + h) * 64
                    A("pool", lambda e, bd=bd, src_w=src_w, hp=hp, h=h, r0=r0: e.dma_start(
                        out=bd[hp:hp + 64, h // 2, hp:hp + 64], in_=src_w[r0:r0 + 64, :]), r=[nm], w=[nm], dma=nm)
            A("act", lambda e: e.activation(out=lc[:, 0:4], in_=pp2[:, PP_LAM:PP_LAM + 4], func=AF.Exp, scale=-1.0), r=["ppt"], w=["lc"])
            A("act", lambda e: e.activation(out=lc[:, 0:4], in_=lc[:, 0:4], func=AF.Ln, scale=1.0, bias=1.0), r=["lc"], w=["lc"])
            A("dve", lambda e: e.tensor_scalar(out=lc[:, 4:8], in0=lc[:, 0:4], scalar1=-LRU_C, scalar2=None, op0=ALU.mult), r=["lc"], w=["lc"])
            A("dve", lambda e: e.memset(histA[:], 0.0), w=["histA"])
            A("dve", lambda e: e.memset(histC[:], 0.0), w=["histC"])
            A("dve", lambda e: e.memset(hst[:], 0.0), w=["hst"])
            A("dve", lambda e: e.memset(gate_sb[:], -1e30), w=["gate_sb"])
            A("dve", lambda e: e.memset(bias_sb[:], 0.0), w=["bias_sb"])

            for c in range(NCH):
                t0 = c * 512
                stage[0] = 1

                A("sp", lambda e, t0=t0, src=src: e.dma_start(out=xt[:], in_=src[t0:t0 + 512, :].rearrange("(s p) d -> p s d", p=128)),
                  r=[f"{srcn}{c}"], w=["xt"], dma="xt")
                A("sp", lambda e, t0=t0: e.dma_start(out=cs[:], in_=rope[t0:t0 + 512, :].rearrange("(s p) d -> p s d", p=128)),
                  w=["cs"], dma="cs")
                rmsnorm(PP_GMIX, False)

                def wslab(j):
                    return load_slab(w_in[l * 1024:(l + 1) * 1024, j * 512:(j + 1) * 512].rearrange("(k p) c -> p k c", p=128), (8, 512))

                stage[0] = 2

                sxa, nxa = wslab(0)
                sba, nba = wslab(1)
                sca, nca = wslab(2)
                for i in range(4):
                    fs = slice(i * 128, (i + 1) * 128)
                    pxa, pxan = mm.get()
                    mm_group(pxa[:], [(sxa[:, k, fs], hT[:, k, :]) for k in range(8)], r=[nxa] + HT, w=[pxan])
                    pca, pcan = mm.get()
                    mm_group(pca[:], [(sca[:, k, fs], hT[:, k, :]) for k in range(8)], r=[nca] + HT, w=[pcan])
                    pba, pban = mm.get()
                    mm_group(pba[:], [(sba[:, k, fs], hT[:, k, :]) for k in range(8)], r=[nba] + HT, w=[pban])
                    xa_sb, xan = wk.get()
                    A("act", lambda e, xa_sb=xa_sb, pxa=pxa: e.copy(out=xa_sb[:], in_=pxa[:]), r=[pxan], w=[xan])
                    A("dve", lambda e, i=i: e.tensor_copy(out=ext[:, 0:2], in_=histA[:, i, :]), r=["histA"], w=["ext"])
                    A("dve", lambda e, xa_sb=xa_sb, pca=pca: e.tensor_tensor(out=ext[:, 2:514], in0=pca[:], in1=xa_sb[:], op=ALU.mult),
                      r=[pcan, xan, "ext"], w=["ext"])
                    A("dve", lambda e, i=i: e.tensor_copy(out=histA[:, i, :], in_=ext[:, 512:514]), r=["ext"], w=["histA"])
                    t1, t1n = wk.get()
                    cw = PP_CWA + i * 3
                    A("dve", lambda e, t1=t1, cw=cw: e.tensor_scalar(out=t1[:], in0=ext[:, 2:514], scalar1=col(cw + 2), scalar2=None, op0=ALU.mult),
                      r=["ext", "ppt"], w=[t1n])
                    A("dve", lambda e, t1=t1, cw=cw: e.scalar_tensor_tensor(out=t1[:], in0=ext[:, 1:513], scalar=col(cw + 1), in1=t1[:],
                                                                          op0=ALU.mult, op1=ALU.add), r=["ext", "ppt", t1n], w=[t1n])
                    A("dve", lambda e, t1=t1, cw=cw: e.scalar_tensor_tensor(out=t1[:], in0=ext[:, 0:512], scalar=col(cw), in1=t1[:],
                                                                          op0=ALU.mult, op1=ALU.add), r=["ext", "ppt", t1n], w=[t1n])
                    A("dve", lambda e, t1=t1, pba=pba, i=i: e.tensor_tensor(out=zT[0][:, i, :], in0=pba[:], in1=t1[:], op=ALU.mult),
                      r=[pban, t1n], w=[f"zA{i}"])

                stage[0] = 3

                def qk_proc(ps, psn, goff, dst_fn, dstn_fn, s):
                    sq, sqn = wk.get()
                    A("act", lambda e: e.activation(out=sq[:], in_=ps[:], func=AF.Square), r=[psn], w=[sqn])
                    A("dve", lambda e: e.tensor_reduce(out=sm[:, 16:24], in_=sq[:].rearrange("p (h d) -> p h d", h=8), axis=AX.X, op=ALU.add),
                      r=[sqn], w=["smq"])
                    A("act", lambda e: e.activation(out=sm[:, 16:24], in_=sm[:, 16:24], func=AF.Sqrt, scale=1.0 / 64, bias=EPS), r=["smq"], w=["smq"])
                    A("dve", lambda e: e.reciprocal(out=sm[:, 16:24], in_=sm[:, 16:24]), r=["smq"], w=["smq"])
                    qg, qgn = wk.get()
                    A("dve", lambda e: e.tensor_tensor(out=qg[:], in0=ps[:], in1=gqk_sb[:, goff:goff + 512], op=ALU.mult), r=[psn, "gqk"], w=[qgn])
                    q3 = qg[:].rearrange("p (h d) -> p h d", h=8)
                    rsb = sm[:, 16:24].rearrange("p (h o) -> p h o", o=1).to_broadcast([128, 8, 64])
                    A("dve", lambda e: e.tensor_tensor(out=q3, in0=q3, in1=rsb, op=ALU.mult), r=[qgn, "smq"], w=[qgn])
                    cosv = cs[:, s, 0:64].rearrange("p (h j) -> p h j", h=8)
                    sinv = cs[:, s, 64:128].rearrange("p (h j) -> p h j", h=8)
                    r1 = q3[:, :, 0:8]
                    r2 = q3[:, :, 8:16]
                    tt, ttn = wk.get()
                    tv = tt[:, 0:256].rearrange("p (a h j) -> p a h j", a=4, h=8)
                    A("dve", lambda e: e.tensor_tensor(out=tv[:, 0], in0=r1, in1=cosv, op=ALU.mult), r=[qgn, "cs"], w=[ttn])
                    A("dve", lambda e: e.tensor_tensor(out=tv[:, 1], in0=r2, in1=sinv, op=ALU.mult), r=[qgn, "cs", ttn], w=[ttn])
                    A("dve", lambda e: e.tensor_tensor(out=tv[:, 2], in0=r2, in1=cosv, op=ALU.mult), r=[qgn, "cs", ttn], w=[ttn])
                    A("dve", lambda e: e.tensor_tensor(out=tv[:, 3], in0=r1, in1=sinv, op=ALU.mult), r=[qgn, "cs", ttn], w=[ttn])
                    o3 = qn_bf[:].rearrange("p (h d) -> p h d", h=8)
                    A("dve", lambda e: e.tensor_tensor(out=o3[:, :, 0:8], in0=tv[:, 0], in1=tv[:, 1], op=ALU.subtract), r=[ttn], w=["qn_bf"])
                    A("dve", lambda e: e.tensor_tensor(out=o3[:, :, 8:16], in0=tv[:, 2], in1=tv[:, 3], op=ALU.add), r=[ttn, "qn_bf"], w=["qn_bf"])
                    A("act", lambda e: e.copy(out=o3[:, :, 16:64], in_=q3[:, :, 16:64]), r=[qgn, "qn_bf"], w=["qn_bf"])
                    tpb, tpn = tp.get()

                    def fn(e):
                        ins = None
                        for pr in range(4):
                            ins = e.transpose(out=tpb[:, pr * 128:(pr + 1) * 128], in_=qn_bf[:, pr * 128:(pr + 1) * 128], identity=id_bf[:])
                        return ins
                    A("pe", fn, r=["qn_bf", "id_bf"], w=[tpn])
                    tpv = tpb[:, 0:512].rearrange("p (a t) -> p a t", a=4)
                    if dst_fn is None:
                        qv = qT[:, :, s * 128:(s + 1) * 128].rearrange("p (a two) t -> p a two t", two=2)
                        A("act", lambda e: e.copy(out=qv[0:64, :, 0, :], in_=tpv[0:64]), r=[tpn], w=[dstn_fn])
                        A("act", lambda e: e.copy(out=qv[64:128, :, 1, :], in_=tpv[64:128]), r=[tpn, dstn_fn], w=[dstn_fn])
                    else:
                        A("act", lambda e: e.copy(out=dst_fn, in_=tpv), r=[tpn], w=[dstn_fn])

                sq_, nq_ = wslab(3)
                sk_, nk_ = wslab(4)
                sv_, nv_ = wslab(5)
                for s in range(4):
                    t = c * 4 + s
                    tsl = slice(s * 128, (s + 1) * 128)
                    pq, pqn = mm.get()
                    mm_group(pq[:], [(hT[:, k, tsl], sq_[:, k, :]) for k in range(8)], r=[nq_, f"hT{s}"], w=[pqn])
                    qk_proc(pq, pqn, 0, None, f"qT{s}", s)
                    pk, pkn = mm.get()
                    mm_group(pk[:], [(hT[:, k, tsl], sk_[:, k, :]) for k in range(8)], r=[nk_, f"hT{s}"], w=[pkn])
                    qk_proc(pk, pkn, 512, kT[:, :, t * 128:(t + 1) * 128], KTN(t), s)
                    pv, pvn = mm.get()
                    mm_group(pv[:], [(hT[:, k, tsl], sv_[:, k, :]) for k in range(8)], r=[nv_, f"hT{s}"], w=[pvn])
                    A("act", lambda e, pv=pv, t=t: e.copy(out=v_all[:, t, :], in_=pv[:]), r=[pvn], w=[VN(t)])
                for bb in range(2):
                    blk = c * 2 + bb
                    A("dve", lambda e, blk=blk: e.tensor_reduce(out=kmean_f[:, :, blk], in_=kT[:, :, blk * 256:(blk + 1) * 256], axis=AX.X, op=ALU.add),
                      r=[KTN(2 * blk), KTN(2 * blk + 1)], w=["kmean_f"])
                    A("act", lambda e, blk=blk: e.mul(out=kmean_bf[:, :, blk], in_=kmean_f[:, :, blk], mul=1.0 / 256), r=["kmean_f"], w=["kmean_bf"])

                stage[0] = 4

                sxc, nxc = wslab(6)
                sgc, ngc = wslab(7)
                for i in range(4):
                    fs = slice(i * 128, (i + 1) * 128)
                    pxc, pxcn = mm.get()
                    mm_group(pxc[:], [(sxc[:, k, fs], hT[:, k, :]) for k in range(8)], r=[nxc] + HT, w=[pxcn])
                    A("dve", lambda e, i=i: e.tensor_copy(out=ext[:, 0:3], in_=histC[:, i, :]), r=["histC"], w=["ext"])
                    A("act", lambda e, pxc=pxc: e.copy(out=ext[:, 3:515], in_=pxc[:]), r=[pxcn, "ext"], w=["ext"])
                    A("dve", lambda e, i=i: e.tensor_copy(out=histC[:, i, :], in_=ext[:, 512:515]), r=["ext"], w=["histC"])
                    xcc, xcn = wk.get()
                    cw = PP_CWC + i * 4
                    A("dve", lambda e, xcc=xcc, cw=cw, i=i: e.tensor_scalar(out=xcc[:], in0=ext[:, 3:515], scalar1=col(cw + 3), scalar2=col(PP_CBC + i),
                                                                          op0=ALU.mult, op1=ALU.add), r=["ext", "ppt"], w=[xcn])
                    for kk in range(3):
                        A("dve", lambda e, xcc=xcc, cw=cw, kk=kk: e.scalar_tensor_tensor(out=xcc[:], in0=ext[:, kk:kk + 512], scalar=col(cw + kk), in1=xcc[:],
                                                                                      op0=ALU.mult, op1=ALU.add), r=["ext", "ppt", xcn], w=[xcn])
                    xcb, xcbn = wkb.get()
                    A("act", lambda e, xcb=xcb, xcc=xcc: e.copy(out=xcb[:], in_=xcc[:]), r=[xcn], w=[xcbn])
                    pr_, prn = mm.get()
                    mm_group(pr_[:], [(bd_r[:, i, :], xcb[:])], r=["bd_r", xcbn], w=[prn])
                    pi_, pin = mm.get()
                    mm_group(pi_[:], [(bd_i[:, i, :], xcb[:])], r=["bd_i", xcbn], w=[pin])
                    rr, rrn = wk.get()
                    A("act", lambda e, rr=rr, pr_=pr_, i=i: e.activation(out=rr[:], in_=pr_[:], func=AF.Sigmoid, bias=col(PP_BR + i), scale=1.0), r=[prn, "ppt"], w=[rrn])
                    ig, ign = wk.get()
                    A("act", lambda e, ig=ig, pi_=pi_, i=i: e.activation(out=ig[:], in_=pi_[:], func=AF.Sigmoid, bias=col(PP_BI + i), scale=1.0), r=[pin, "ppt"], w=[ign])
                    A("dve", lambda e, rr=rr, i=i: e.tensor_scalar(out=rr[:], in0=rr[:], scalar1=lc[:, 4 + i:5 + i], scalar2=None, op0=ALU.mult), r=[rrn, "lc"], w=[rrn])
                    aa, aan = wk.get()
                    A("act", lambda e, aa=aa, rr=rr: e.activation(out=aa[:], in_=rr[:], func=AF.Exp), r=[rrn], w=[aan])
                    A("act", lambda e, rr=rr: e.activation(out=rr[:], in_=rr[:], func=AF.Exp, scale=2.0), r=[rrn], w=[rrn])
                    A("act", lambda e, rr=rr: e.activation(out=rr[:], in_=rr[:], func=AF.Sqrt, scale=-1.0, bias=1.0), r=[rrn], w=[rrn])
                    A("dve", lambda e, ig=ig, xcc=xcc: e.tensor_tensor(out=ig[:], in0=ig[:], in1=xcc[:], op=ALU.mult), r=[ign, xcn], w=[ign])
                    A("dve", lambda e, ig=ig, rr=rr: e.tensor_tensor(out=ig[:], in0=ig[:], in1=rr[:], op=ALU.mult), r=[ign, rrn], w=[ign])
                    A("dve", lambda e, xcc=xcc, aa=aa, ig=ig, i=i: e.tensor_tensor_scan(out=xcc[:], data0=aa[:], data1=ig[:], initial=hst[:, i:i + 1],
                                                                                     op0=ALU.mult, op1=ALU.add), r=[aan, ign, "hst", xcn], w=[xcn])
                    A("dve", lambda e, xcc=xcc, i=i: e.tensor_copy(out=hst[:, i:i + 1], in_=xcc[:, 511:512]), r=[xcn], w=["hst"])
                    pg, pgn = mm.get()
                    mm_group(pg[:], [(sgc[:, k, fs], hT[:, k, :]) for k in range(8)], r=[ngc] + HT, w=[pgn])
                    A("act", lambda e, aa=aa, pg=pg: e.activation(out=aa[:], in_=pg[:], func=AF.Gelu_apprx_tanh), r=[pgn, aan], w=[aan])
                    A("dve", lambda e, aa=aa, xcc=xcc, i=i: e.tensor_tensor(out=zT[2][:, i, :], in0=aa[:], in1=xcc[:], op=ALU.mult), r=[aan, xcn], w=[f"zC{i}"])

                stage[0] = 5
                for s in range(4):
                    t = c * 4 + s
                    b = t // 2
                    half = t % 2
                    tsl = slice(s * 128, (s + 1) * 128)
                    masked = b > MOBA_TOPK
                    if masked:
                        pgl, pgln = mm.get()

                        def fng(e, b=b, tsl=tsl, pgl=pgl):
                            ins = None
                            for h in range(8):
                                ins = e.matmul(pgl[:, h * 16:h * 16 + b], lhsT=qT[:, h, tsl], rhs=kmean_bf[:, h // 2, 0:b],
                                               start=True, stop=True)
                            return ins
                        A("pe", fng, r=[f"qT{s}", "kmean_bf"], w=[pgln])
                        A("dve", lambda e, b=b, pgl=pgl: e.tensor_copy(out=gate_sb[:, :, 0:b], in_=pgl[:, 0:128].rearrange("p (h n) -> p h n", h=8)[:, :, 0:b]),
                          r=[pgln], w=["gate_sb"])
                        for h in range(8):
                            A("dve", lambda e, h=h: e.max(out=m8[:, h * 8:(h + 1) * 8], in_=gate_sb[:, h, :]), r=["gate_sb"], w=["m8"])
                        for h in range(8):
                            A("dve", lambda e, h=h, b=b: e.tensor_scalar(out=bias_sb[:, h, 0:b], in0=gate_sb[:, h, 0:b], scalar1=m8[:, h * 8 + 2:h * 8 + 3],
                                                                        scalar2=NEGB, op0=ALU.is_lt, op1=ALU.mult), r=["gate_sb", "m8"], w=["bias_sb"])
                    units = []
                    for h in range(8):
                        hu = [("past", n, [2 * n, 2 * n + 1]) for n in range(max(0, b - int(os.environ.get("ATT_MAXPAST", 99))), b)]
                        hu.append(("own", b, [2 * b] if half == 0 else [2 * b, 2 * b + 1]))
                        for i, (kind, n, tiles) in enumerate(hu):
                            units.append((h, kind, n, tiles, i == 0, i == len(hu) - 1))

                    def emit_S(u):
                        h, kind, n, tiles, first, last = u
                        pair = h // 2
                        ps, psn = mm.get()
                        PT, PTn = PT_pool.get()
                        q_r = qT[:, h, tsl]
                        nt_ = len(tiles)
                        use_mask = masked and kind == "past"
                        r_s = [f"qT{s}"] + [KTN(kt) for kt in tiles]
                        B2 = None
                        if use_mask:
                            B2, B2n = B2_pool.get()
                            A("dve", lambda e: e.tensor_scalar(out=B2[:], in0=id_bf[:], scalar1=bias_sb[:, h, n:n + 1], scalar2=None, op0=ALU.mult),
                              r=["id_bf", "bias_sb"], w=[B2n])
                            r_s = r_s + [B2n, "ones_bf"]

                        def fn(e):
                            ins = None
                            for j, kt in enumerate(tiles):
                                ins = e.matmul(ps[:, j * 128:(j + 1) * 128], lhsT=kT[:, pair, kt * 128:(kt + 1) * 128], rhs=q_r,
                                               start=True, stop=not use_mask)
                                if use_mask:
                                    ins = e.matmul(ps[:, j * 128:(j + 1) * 128], lhsT=ones_bf[:], rhs=B2[:], start=False, stop=True)
                            return ins
                        A("pe", fn, r=r_s, w=[psn])
                        if kind == "past":
                            A("act", lambda e: e.activation(out=PT[:, 0:256], in_=ps[:, 0:256], func=AF.Exp, scale=0.125), r=[psn], w=[PTn])
                        else:
                            dj = nt_ - 1
                            if nt_ == 2:
                                A("act", lambda e: e.activation(out=PT[:, 0:128], in_=ps[:, 0:128], func=AF.Exp, scale=0.125), r=[psn], w=[PTn])
                            dt_, dtn = dtmp.get()
                            A("dve", lambda e: e.scalar_tensor_tensor(out=dt_[:], in0=ps[:, dj * 128:(dj + 1) * 128], scalar=0.125, in1=triT[:],
                                                                      op0=ALU.mult, op1=ALU.add), r=[psn, "triT"], w=[dtn])
                            A("act", lambda e: e.activation(out=PT[:, dj * 128:(dj + 1) * 128], in_=dt_[:], func=AF.Exp),
                              r=[dtn] + ([PTn] if nt_ == 2 else []), w=[PTn])
                        return (u, PT, PTn)

                    def emit_PV(item):
                        u, PT, PTn = item
                        h, kind, n, tiles, first, last = u
                        pair = h // 2
                        hp = (h % 2) * 64
                        nt_ = len(tiles)
                        k = pvc[0] % 2
                        pvc[0] += 1
                        po, pd = psOut[k], psDen[k]
                        pon, pdn = f"pso{k}", f"psd{k}"

                        def fnV(e):
                            ins = None
                            for j, kt in enumerate(tiles):
                                e.matmul(po[:, 0:128], lhsT=v_all[:, kt, pair * 128:(pair + 1) * 128], rhs=PT[:, j * 128:(j + 1) * 128],
                                         start=(j == 0), stop=(j == nt_ - 1))
                                ins = e.matmul(pd[:, 0:128], lhsT=ones_bf[:], rhs=PT[:, j * 128:(j + 1) * 128], start=(j == 0), stop=(j == nt_ - 1))
                            return ins
                        A("pe", fnV, r=[PTn, "ones_bf"] + [VN(kt) for kt in tiles], w=[pon, pdn])
                        ao, ad = accO[h % 2], accD[h % 2]
                        aon, adn = f"accO{h % 2}", f"accD{h % 2}"
                        if first:
                            A("dve", lambda e: e.tensor_copy(out=ao[hp:hp + 64, :], in_=po[hp:hp + 64, 0:128]), r=[pon], w=[aon])
                            A("dve", lambda e: e.tensor_copy(out=ad[hp:hp + 64, :], in_=pd[hp:hp + 64, 0:128]), r=[pdn], w=[adn])
                        else:
                            A("dve", lambda e: e.tensor_tensor(out=ao[hp:hp + 64, :], in0=po[hp:hp + 64, 0:128], in1=ao[hp:hp + 64, :], op=ALU.add),
                              r=[pon, aon], w=[aon])
                            A("dve", lambda e: e.tensor_tensor(out=ad[hp:hp + 64, :], in0=pd[hp:hp + 64, 0:128], in1=ad[hp:hp + 64, :], op=ALU.add),
                              r=[pdn, adn], w=[adn])
                        if last:
                            zdst = zT[1][hp:hp + 64, pair, tsl]
                            A("dve", lambda e: e.reciprocal(out=ad[hp:hp + 64, :], in_=ad[hp:hp + 64, :]), r=[adn], w=[adn])
                            A("dve", lambda e: e.tensor_tensor(out=zdst, in0=ao[hp:hp + 64, :], in1=ad[hp:hp + 64, :], op=ALU.mult),
                              r=[aon, adn], w=[f"zB{s}"])

                    prev = None
                    for u in units:
                        item = emit_S(u)
                        if prev is not None:
                            emit_PV(prev)
                        prev = item
                    emit_PV(prev)
                stage[0] = 5.8
                if dbg:
                    for (zi, nm, rd) in ((0, "d_za", [f"zA{i}" for i in range(4)]), (1, "d_zb", [f"zB{i}" for i in range(4)]), (2, "d_zc", [f"zC{i}" for i in range(4)])):
                        if l == 0:
                            A("sp", lambda e, zi=zi, nm=nm, t0=t0: e.dma_start(out=dbg_out[nm][:, t0:t0 + 512].rearrange("(a p) t -> p a t", p=128), in_=zT[zi][:]),
                              r=rd, w=[nm], dma="dbg" + nm)

                stage[0] = 6

                ZN = [[f"zA{i}" for i in range(4)], [f"zB{i}" for i in range(4)], [f"zC{i}" for i in range(4)]]
                for mq in range(2):
                    for br in range(3):
                        wsl, wn = load_slab(wo_abc[br][l * 512:(l + 1) * 512, mq * 512:(mq + 1) * 512].rearrange("(k p) c -> p k c", p=128), (4, 512))
                        gsl, gn = wslab(8 + br * 2 + mq)
                        for mi in range(4):
                            m = mq * 4 + mi
                            fs = slice(mi * 128, (mi + 1) * 128)
                            py, pyn = mm.get()
                            mm_group(py[:], [(wsl[:, k, fs], zT[br][:, k, :]) for k in range(4)], r=[wn] + ZN[br], w=[pyn])
                            pgt, pgtn = mm.get()
                            mm_group(pgt[:], [(gsl[:, k, fs], hT[:, k, :]) for k in range(8)], r=[gn] + HT, w=[pgtn])
                            sg, sgn = wk.get()
                            A("act", lambda e, sg=sg, pgt=pgt: e.activation(out=sg[:], in_=pgt[:], func=AF.Sigmoid), r=[pgtn], w=[sgn])
                            if br == 0:
                                A("dve", lambda e, sg=sg, py=py, mi=mi: e.tensor_tensor(out=macc[:, mi, :], in0=py[:], in1=sg[:], op=ALU.mult), r=[pyn, sgn], w=[f"macc{mi}"])
                            else:
                                A("dve", lambda e, sg=sg, py=py: e.tensor_tensor(out=sg[:], in0=py[:], in1=sg[:], op=ALU.mult), r=[pyn, sgn], w=[sgn])
                                if br == 1:
                                    A("dve", lambda e, sg=sg, mi=mi: e.tensor_tensor(out=macc[:, mi, :], in0=macc[:, mi, :], in1=sg[:], op=ALU.add), r=[sgn, f"macc{mi}"], w=[f"macc{mi}"])
                                else:
                                    A("dve", lambda e, sg=sg, mi=mi, m=m: e.tensor_tensor(out=mergedT[:, m, :], in0=macc[:, mi, :], in1=sg[:], op=ALU.add), r=[sgn, f"macc{mi}"], w=[f"mg{m}"])
                MG = [f"mg{m}" for m in range(8)]
                if dbg and l == 0:
                    A("sp", lambda e, t0=t0: e.dma_start(out=dbg_out["d_mg"][:, t0:t0 + 512].rearrange("(a p) t -> p a t", p=128), in_=mergedT[:]),
                      r=MG, w=["d_mg"], dma="dbgmg")
                for hf in range(2):
                    osl, on = load_slab(w_o[l * 1024:(l + 1) * 1024, hf * 512:(hf + 1) * 512].rearrange("(k p) c -> p k c", p=128), (8, 512))
                    for s in range(4):
                        tsl = slice(s * 128, (s + 1) * 128)
                        po, pon = mm.get()
                        mm_group(po[:], [(mergedT[:, k, tsl], osl[:, k, :]) for k in range(8)], r=[on] + MG, w=[pon])
                        A("dve", lambda e, po=po, s=s, hf=hf: e.tensor_tensor(out=xt[:, s, hf * 512:(hf + 1) * 512], in0=po[:], in1=xt[:, s, hf * 512:(hf + 1) * 512], op=ALU.add),
                          r=[pon, "xt"], w=["xt"])
                if dbg and l == 0:
                    A("sp", lambda e, t0=t0: e.dma_start(out=dbg_out["d_xmix"][t0:t0 + 512, :].rearrange("(s p) d -> p s d", p=128), in_=xt[:]),
                      r=["xt"], w=["d_xmix"], dma="dbgx")

                stage[0] = 7

                is_moe = (l % 2 == 1)
                rmsnorm(PP_GFFN, is_moe)
                if not is_moe:
                    groups = [(None, f0, min(4, 22 - f0)) for f0 in range(0, 22, 4)]
                else:
                    groups = [(ex, f0, 4) for ex in range(8) for f0 in range(0, 28, 4)]
                for (ex, f0, nf) in groups:
                    if ex is None:
                        gsrc = ffn_g[:, f0 * 128:(f0 + nf) * 128]
                        usrc = ffn_u[:, f0 * 128:(f0 + nf) * 128]
                        dsrc = ffn_d[f0 * 128:(f0 + nf) * 128, :]
                    else:
                        gsrc = moe_g[ex * 1024:(ex + 1) * 1024, f0 * 128:(f0 + nf) * 128]
                        usrc = moe_u[ex * 1024:(ex + 1) * 1024, f0 * 128:(f0 + nf) * 128]
                        dsrc = moe_d[ex * 3584 + f0 * 128:ex * 3584 + (f0 + nf) * 128, :]
                    gsl, gn = load_slab(gsrc.rearrange("(k p) c -> p k c", p=128), (8, nf * 128))
                    usl, un = load_slab(usrc.rearrange("(k p) c -> p k c", p=128), (8, nf * 128))
                    dsl, dn = load_slab(dsrc.rearrange("(f p) c -> p f c", p=128), (nf, 1024))
                    aT, aTn = aT_pool.get()
                    for f in range(nf):
                        fs = slice(f * 128, (f + 1) * 128)
                        pgg, pggn = mm.get()
                        mm_group(pgg[:], [(gsl[:, k, fs], hT[:, k, :]) for k in range(8)], r=[gn] + HT, w=[pggn])
                        puu, puun = mm.get()
                        mm_group(puu[:], [(usl[:, k, fs], hT[:, k, :]) for k in range(8)], r=[un] + HT, w=[puun])
                        sg, sgn = wk.get()
                        A("act", lambda e, sg=sg, pgg=pgg: e.activation(out=sg[:], in_=pgg[:], func=AF.Silu), r=[pggn], w=[sgn])
                        A("dve", lambda e, sg=sg, puu=puu, aT=aT, f=f: e.tensor_tensor(out=aT[:, f, :], in0=puu[:], in1=sg[:], op=ALU.mult), r=[puun, sgn], w=[f"{aTn}_{f}"])
                    aN = [f"{aTn}_{f}" for f in range(nf)]
                    for s in range(4):
                        tsl = slice(s * 128, (s + 1) * 128)
                        for hf in range(2):
                            pd, pdn = mm.get()
                            mm_group(pd[:], [(aT[:, f, tsl], dsl[:, f, hf * 512:(hf + 1) * 512]) for f in range(nf)], r=[dn] + aN, w=[pdn])
                            xv = xt[:, s, hf * 512:(hf + 1) * 512]
                            if ex is None:
                                A("dve", lambda e, pd=pd, xv=xv: e.tensor_tensor(out=xv, in0=pd[:], in1=xv, op=ALU.add), r=[pdn, "xt"], w=["xt"])
                            else:
                                A("dve", lambda e, pd=pd, xv=xv, s=s, ex=ex: e.scalar_tensor_tensor(out=xv, in0=pd[:], scalar=comb[:, s, ex:ex + 1], in1=xv,
                                                                                                 op0=ALU.mult, op1=ALU.add), r=[pdn, "xt", "comb"], w=["xt"])
                stage[0] = 0
                A("sp", lambda e, t0=t0, dst=dst: e.dma_start(out=dst[t0:t0 + 512, :].rearrange("(s p) d -> p s d", p=128), in_=xt[:]),
                  r=["xt"], w=[f"{dstn}{c}"], dma="xt_st")
        fin = [f"y{c}" for c in range(NCH)] + (list(dbg_out.keys()) if dbg else [])
        A("sp", lambda e: e.nop(), r=fin)
        Sc.emit(st)
        nc._n_ops = len(Sc.ops)
    return nc


def host_prep(inputs, S=4096):
    f = lambda k: np.asarray(inputs[k], dtype=np.float32)
    L = 2
    pp = np.zeros((L, 128, 64), np.float32)
    for l in range(L):
        pp[l, :, PP_GMIX:PP_GMIX + 8] = f("norm_mix")[l].reshape(8, 128).T
        pp[l, :, PP_GFFN:PP_GFFN + 8] = f("norm_ffn")[l].reshape(8, 128).T
        ca = f("conv_a_w")[l]
        pp[l, :, PP_CWA:PP_CWA + 12] = ca.reshape(3, 4, 128).transpose(2, 1, 0).reshape(128, 12)
        cc = f("conv_c_w")[l]
        pp[l, :, PP_CWC:PP_CWC + 16] = cc.reshape(4, 4, 128).transpose(2, 1, 0).reshape(128, 16)
        pp[l, :, PP_CBC:PP_CBC + 4] = f("conv_c_b")[l].reshape(4, 128).T
        pp[l, :, PP_BR:PP_BR + 4] = f("rg_b_r")[l].reshape(4, 128).T
        pp[l, :, PP_BI:PP_BI + 4] = f("rg_b_i")[l].reshape(4, 128).T
        pp[l, :, PP_LAM:PP_LAM + 4] = f("rg_lambda")[l].reshape(4, 128).T
    gqk = np.zeros((L, 128, 1024), np.float32)
    for l in range(L):
        gqk[l, :, 0:512] = np.tile(f("q_norm")[l], 8)[None, :]
        gqk[l, :, 512:1024] = np.tile(f("k_norm")[l], 8)[None, :]
    half = ROT_DIM // 2
    inv_freq = (ROPE_THETA ** (-np.arange(0, ROT_DIM, 2, dtype=np.float32) / ROT_DIM)).astype(np.float32)
    ang = np.arange(S, dtype=np.float32)[:, None] * inv_freq[None, :]
    rope = np.concatenate([np.tile(np.cos(ang), (1, 8)), np.tile(np.sin(ang), (1, 8))], axis=1).astype(np.float32)
    shared = {
        "w_in": f("w_in").reshape(2 * 1024, 7168),
        "wo_a": f("wo_a").reshape(2 * 512, 1024), "wo_b": f("wo_b").reshape(2 * 512, 1024), "wo_c": f("wo_c").reshape(2 * 512, 1024),
        "w_o": f("w_o").reshape(2 * 1024, 1024),
        "ffn_w_gate": f("ffn_w_gate").reshape(1024, 2816), "ffn_w_up": f("ffn_w_up").reshape(1024, 2816),
        "ffn_w_down": f("ffn_w_down").reshape(2816, 1024),
        "moe_router": f("moe_router").reshape(1024, 8),
        "moe_w_gate": f("moe_w_gate").reshape(8 * 1024, 3584), "moe_w_up": f("moe_w_up").reshape(8 * 1024, 3584),
        "moe_w_down": f("moe_w_down").reshape(8 * 3584, 1024),
        "rg_w_r": f("rg_w_r").reshape(2 * 8 * 64, 64), "rg_w_i": f("rg_w_i").reshape(2 * 8 * 64, 64),
        "pp": pp.reshape(2 * 128, 64), "gqk": gqk.reshape(2 * 128, 1024), "rope": rope,
    }
    return shared


_NC_CACHE = {}


def kernel(**inputs):
    x = np.asarray(inputs["x"], dtype=np.float32)
    B, S, D = x.shape
    shared = host_prep(inputs, S)
    if S not in _NC_CACHE:
        _NC_CACHE[S] = build_nc(S=S)
    nc = _NC_CACHE[S]
    in_maps = [dict(shared, x=np.ascontiguousarray(x[b])) for b in range(B)]
    res = run_bass_kernel_spmd(nc, in_maps, core_ids=list(range(B)))
    return np.stack([np.asarray(r["y"], dtype=np.float32) for r in res.results], axis=0)
```

```python
import os
import numpy as np
from contextlib import ExitStack
import concourse.bass as bass
import concourse.mybir as mybir
from concourse.bass_utils import run_bass_kernel_spmd

F32 = mybir.dt.float32
BF16 = mybir.dt.bfloat16
AF = mybir.ActivationFunctionType
ALU = mybir.AluOpType
AX = mybir.AxisListType

SEM_LIMIT = 30000
EPS = 1e-6
LRU_C = 8.0
MOBA_TOPK = 3
ROT_DIM = 16
ROPE_THETA = 500000.0


class Sched:
    ENGS = ("pe", "act", "dve", "pool", "sp")

    def __init__(self, nc):
        self.nc = nc
        self.ops = []

    def add(self, eng, fn, reads=(), writes=(), dma=None):
        assert eng in self.ENGS
        self.ops.append(dict(eng=eng, fn=fn, reads=tuple(reads), writes=tuple(writes), dma=dma,
                             deps=set(), signal=False))
        return len(self.ops) - 1

    def analyze(self):
        last_w = {}
        readers = {}
        for i, op in enumerate(self.ops):
            deps = set()
            for r in op["reads"]:
                if r in last_w:
                    deps.add(last_w[r])
            for w in op["writes"]:
                if w in last_w:
                    deps.add(last_w[w])
                for rd in readers.get(w, ()):
                    deps.add(rd)
            deps.discard(i)
            op["deps"] = deps
            for r in op["reads"]:
                readers.setdefault(r, []).append(i)
            for w in op["writes"]:
                last_w[w] = i
                readers[w] = []
        for i, op in enumerate(self.ops):
            if op["eng"] == "pe" and op["dma"] is None:
                op["deps"] = {d for d in op["deps"]
                              if not (self.ops[d]["eng"] == "pe" and self.ops[d]["dma"] is None)}
        for op in self.ops:
            if op["dma"] is not None:
                op["signal"] = True
            for d in op["deps"]:
                self.ops[d]["signal"] = True

    def emit(self, stack):
        nc = self.nc
        self.analyze()
        eng_sem = {}
        eng_cnt = {}
        sems = []

        def new_sem(name):
            s = stack.enter_context(nc.semaphore(name))
            sems.append(s)
            return s

        dma_sem = {}
        dma_cnt = {}
        for e in self.ENGS:
            eng_sem[e] = new_sem(f"s_{e}_0")
            eng_cnt[e] = 0
        for i, op in enumerate(self.ops):
            if not op["signal"]:
                op["sig"] = None
                continue
            if op["dma"] is not None:
                k = op["dma"]
                if k not in dma_sem or dma_cnt[k] + 16 > SEM_LIMIT:
                    dma_sem[k] = new_sem(f"d{len(sems)}")
                    dma_cnt[k] = 0
                dma_cnt[k] += 16
                op["sig"] = (dma_sem[k], dma_cnt[k], 16)
            else:
                e = op["eng"]
                if eng_cnt[e] + 1 > SEM_LIMIT:
                    eng_sem[e] = new_sem(f"s_{e}_{i}")
                    eng_cnt[e] = 0
                eng_cnt[e] += 1
                op["sig"] = (eng_sem[e], eng_cnt[e], 1)
        self.n_sems = len(sems)
        block = stack.enter_context(nc.Block())
        ops = self.ops

        def make(engname):
            def body(eng):
                known = {}
                for op in ops:
                    if op["eng"] != engname:
                        continue
                    for d in sorted(op["deps"]):
                        sem, val, _ = ops[d]["sig"]
                        key = id(sem)
                        if known.get(key, 0) >= val:
                            continue
                        eng.wait_ge(sem, val)
                        known[key] = val
                    ins = op["fn"](eng)
                    if op["sig"] is not None:
                        sem, val, inc = op["sig"]
                        ins.then_inc(sem, inc)
            return body

        block.tensor(make("pe"))
        block.scalar(make("act"))
        block.vector(make("dve"))
        block.gpsimd(make("pool"))
        block.sync(make("sp"))


class RPool:
    def __init__(self, name, tiles):
        self.name = name
        self.tiles = tiles
        self.i = 0

    def get(self):
        k = self.i % len(self.tiles)
        self.i += 1
        return self.tiles[k], f"{self.name}{k}"


PP_GMIX, PP_GFFN, PP_CWA, PP_CWC, PP_CBC, PP_BR, PP_BI, PP_LAM = 0, 8, 16, 28, 44, 48, 52, 56
NEGM = -30000.0
NEGB = -240000.0


def build_nc(S=4096, L=2, dbg=False, upto=99):
    nc = bass.Bass("TRN2", target_bir_lowering=False)
    NCH = S // 512
    NT = S // 128
    NB = S // 256
    D = 1024

    def din(name, shape, dt=F32):
        return nc.dram_tensor(name, shape, dt, kind="ExternalInput").ap()

    x = din("x", [S, D])
    w_in = din("w_in", [2 * 1024, 7168])
    wo_abc = [din("wo_a", [2 * 512, 1024]), din("wo_b", [2 * 512, 1024]), din("wo_c", [2 * 512, 1024])]
    w_o = din("w_o", [2 * 1024, 1024])
    ffn_g = din("ffn_w_gate", [1024, 2816])
    ffn_u = din("ffn_w_up", [1024, 2816])
    ffn_d = din("ffn_w_down", [2816, 1024])
    moe_r = din("moe_router", [1024, 8])
    moe_g = din("moe_w_gate", [8 * 1024, 3584])
    moe_u = din("moe_w_up", [8 * 1024, 3584])
    moe_d = din("moe_w_down", [8 * 3584, 1024])
    rg_r = din("rg_w_r", [2 * 8 * 64, 64])
    rg_i = din("rg_w_i", [2 * 8 * 64, 64])
    pp = din("pp", [2 * 128, 64])
    gqk = din("gqk", [2 * 128, 1024])
    rope = din("rope", [S, 128])
    y = nc.dram_tensor("y", [S, D], F32, kind="ExternalOutput").ap()
    xmid = nc.dram_tensor("xmid", [S, D], F32, kind="Internal").ap()
    dbg_out = {}
    if dbg:
        for nm, shp in (("d_za", [512, S]), ("d_zb", [512, S]), ("d_zc", [512, S]), ("d_mg", [1024, S]),
                        ("d_xmix", [S, 1024])):
            dbg_out[nm] = nc.dram_tensor(nm, shp, F32 if nm == "d_xmix" else BF16, kind="ExternalOutput").ap()

    with ExitStack() as st:
        def sb(name, shape, dt=F32):
            return st.enter_context(nc.sbuf_tensor(name, shape, dt))

        def psum(name, shape, dt=F32):
            return st.enter_context(nc.psum_tensor(name, shape, dt))

        Sc = Sched(nc)

        stage = [0]

        def A(eng, fn, r=(), w=(), dma=None):
            if stage[0] <= upto:
                Sc.add(eng, fn, reads=r, writes=w, dma=dma)

        qn_bf = sb("qn_bf", [128, 512], BF16)
        id_f = sb("id_f", [128, 128])
        id_bf = sb("id_bf", [128, 128], BF16)
        triT = sb("triT", [128, 128])
        ones_bf = sb("ones_bf", [128, 128], BF16)
        ppt = sb("ppt", [128, 64])
        pp2 = ppt[:]
        gqk_sb = sb("gqk_sb", [128, 1024])
        bd_r = sb("bd_r", [128, 4, 128], BF16)
        bd_i = sb("bd_i", [128, 4, 128], BF16)
        R_sb = sb("R_sb", [128, 8, 8])
        lc = sb("lc", [128, 12])
        xt = sb("xt", [128, 4, 1024])
        junk = sb("junk", [128, 1024], BF16)
        ss = sb("ss", [128, 8])
        xs_pool = RPool("xs", [sb(f"xs{i}", [128, 1024]) for i in range(1)])
        h2f = sb("h2f", [128, 8, 128])
        hT = sb("hT", [128, 8, 512], BF16)
        ws = RPool("ws", [sb(f"ws{i}", [128, 4096], BF16) for i in range(4)])
        zT = [sb(f"zT{i}", [128, 4, 512], BF16) for i in range(3)]
        macc = sb("macc", [128, 4, 512])
        mergedT = sb("mergedT", [128, 8, 512], BF16)
        aT_pool = RPool("aT", [sb(f"aT{i}", [128, 4, 512], BF16) for i in range(2)])
        kT = sb("kT", [128, 4, S], BF16)
        v_all = sb("v_all", [128, NT, 512], BF16)
        qT = sb("qT", [128, 8, 512], BF16)
        cs = sb("cs", [128, 4, 128])
        histA = sb("histA", [128, 4, 2])
        histC = sb("histC", [128, 4, 3])
        hst = sb("hst", [128, 4])
        ext = sb("ext", [128, 515])
        wk = RPool("wk", [sb(f"wk{i}", [128, 512]) for i in range(5)])
        wkb = RPool("wkb", [sb(f"wkb{i}", [128, 512], BF16) for i in range(2)])
        sm = sb("sm", [128, 64])
        kmean_f = sb("kmean_f", [128, 4, 16])
        kmean_bf = sb("kmean_bf", [128, 4, 16], BF16)
        gate_sb = sb("gate_sb", [128, 8, 16])
        m8 = sb("m8", [128, 64])
        bias_sb = sb("bias_sb", [128, 8, 16])
        PT_pool = RPool("PT", [sb(f"PT{i}", [128, 256], BF16) for i in range(4)])
        B2_pool = RPool("B2", [sb(f"B2{i}", [128, 128], BF16) for i in range(2)])
        dtmp = RPool("dtmp", [sb(f"dtmp{i}", [128, 128]) for i in range(2)])
        comb = sb("comb", [128, 4, 8])
        lg = sb("lg", [128, 8])
        mm = RPool("mm", [psum(f"mm{i}", [128, 512]) for i in range(3)])
        tp = RPool("tp", [psum(f"tp{i}", [128, 1024], BF16) for i in range(1)])
        psOut = [psum("psOa", [128, 512]), psum("psOb", [128, 512])]
        psDen = [psum("psDa", [128, 512]), psum("psDb", [128, 512])]
        accO = [sb(f"accO{i}", [128, 128]) for i in range(2)]
        accD = [sb(f"accD{i}", [128, 128]) for i in range(2)]
        pvc = [0]

        def col(j):
            return pp2[:, j:j + 1]

        A("pool", lambda e: e.memset(id_f[:], 1.0), w=["id_f"])
        A("pool", lambda e: e.affine_select(out=id_f[:], in_=id_f[:], pattern=[[-1, 128]], compare_op=ALU.is_equal,
                                            fill=0.0, base=0, channel_multiplier=1), r=["id_f"], w=["id_f"])
        A("dve", lambda e: e.tensor_copy(out=id_bf[:], in_=id_f[:]), r=["id_f"], w=["id_bf"])
        A("pool", lambda e: e.memset(triT[:], 0.0), w=["triT"])
        A("pool", lambda e: e.affine_select(out=triT[:], in_=triT[:], pattern=[[1, 128]], compare_op=ALU.is_ge,
                                            fill=NEGM, base=0, channel_multiplier=-1), r=["triT"], w=["triT"])
        A("dve", lambda e: e.memset(ones_bf[:], 1.0), w=["ones_bf"])
        A("dve", lambda e: e.memset(qT[:], 0.0), w=["qT0", "qT1", "qT2", "qT3"])
        A("sp", lambda e: e.dma_start(out=R_sb[:], in_=moe_r.rearrange("(k p) e -> p k e", p=128)), w=["R_sb"], dma="R_sb")

        def load_slab(src_ap, shape3):
            t, nm = ws.get()
            a, b = shape3
            v = t[:, 0:a * b].rearrange("p (a b) -> p a b", a=a)
            A("pool", lambda e: e.dma_start(out=v, in_=src_ap), w=[nm], dma=nm)
            return v, nm

        def mm_group(ps_ap, pairs, r, w):
            n = len(pairs)

            def fn(e):
                ins = None
                for i, (lt, rh) in enumerate(pairs):
                    ins = e.matmul(ps_ap, lhsT=lt, rhs=rh, start=(i == 0), stop=(i == n - 1))
                return ins
            A("pe", fn, r=r, w=w)

        def rmsnorm(g0, router):
            for s in range(4):
                A("act", lambda e, s=s: e.activation(out=junk[:], in_=xt[:, s, :], func=AF.Square,
                                                     accum_out=ss[:, s:s + 1]), r=["xt"], w=["junk", "ss"])
            A("act", lambda e: e.activation(out=ss[:, 4:8], in_=ss[:, 0:4], func=AF.Sqrt, scale=1.0 / D, bias=EPS),
              r=["ss"], w=["ss"])
            A("dve", lambda e: e.reciprocal(out=ss[:, 4:8], in_=ss[:, 4:8]), r=["ss"], w=["ss"])
            for s in range(4):
                xs, xsn = xs_pool.get()
                A("dve", lambda e, s=s, xs=xs: e.tensor_scalar(out=xs[:], in0=xt[:, s, :], scalar1=ss[:, 4 + s:5 + s],
                                                              scalar2=None, op0=ALU.mult), r=["xt", "ss"], w=[xsn])
                for hf in range(2):
                    bank, bn = mm.get()

                    def fn(e, xs=xs, bank=bank, hf=hf):
                        ins = None
                        for kk in range(4):
                            k = hf * 4 + kk
                            ins = e.transpose(out=bank[:, kk * 128:(kk + 1) * 128], in_=xs[:, k * 128:(k + 1) * 128],
                                              identity=id_f[:])
                        return ins
                    A("pe", fn, r=[xsn, "id_f"], w=[bn])
                    bv = bank[:].rearrange("p (k t) -> p k t", k=4)
                    gb = pp2[:, g0 + hf * 4:g0 + hf * 4 + 4].rearrange("p (k o) -> p k o", o=1).to_broadcast([128, 4, 128])
                    if router:
                        A("dve", lambda e, bv=bv, gb=gb, hf=hf: e.tensor_tensor(out=h2f[:, hf * 4:hf * 4 + 4, :], in0=bv, in1=gb,
                                                                              op=ALU.mult), r=[bn, "ppt"], w=[f"h2f{hf}"])
                        A("act", lambda e, hf=hf, s=s: e.copy(out=hT[:, hf * 4:hf * 4 + 4, s * 128:(s + 1) * 128],
                                                             in_=h2f[:, hf * 4:hf * 4 + 4, :]), r=[f"h2f{hf}"], w=[f"hT{s}"])
                    else:
                        A("dve", lambda e, bv=bv, gb=gb, hf=hf, s=s: e.tensor_tensor(
                            out=hT[:, hf * 4:hf * 4 + 4, s * 128:(s + 1) * 128], in0=bv, in1=gb, op=ALU.mult),
                          r=[bn, "ppt"], w=[f"hT{s}"])
                if router:
                    pl_, pln = mm.get()
                    mm_group(pl_[:, 0:8], [(h2f[:, k, :], R_sb[:, k, :]) for k in range(8)], r=["h2f0", "h2f1", "R_sb"], w=[pln])
                    A("dve", lambda e, pl_=pl_: e.tensor_copy(out=lg[:], in_=pl_[:, 0:8]), r=[pln], w=["lg"])
                    A("dve", lambda e: e.max(out=m8[:, 0:8], in_=lg[:]), r=["lg"], w=["m8"])
                    A("dve", lambda e: e.tensor_tensor(out=sm[:, 0:1], in0=m8[:, 1:2], in1=m8[:, 0:1], op=ALU.subtract), r=["m8"], w=["sm"])
                    A("act", lambda e: e.activation(out=sm[:, 1:2], in_=sm[:, 0:1], func=AF.Exp), r=["sm"], w=["sm"])
                    A("dve", lambda e: e.tensor_scalar(out=sm[:, 2:3], in0=sm[:, 1:2], scalar1=1.0, scalar2=None, op0=ALU.add), r=["sm"], w=["sm"])
                    A("dve", lambda e: e.reciprocal(out=sm[:, 2:3], in_=sm[:, 2:3]), r=["sm"], w=["sm"])
                    A("dve", lambda e: e.tensor_tensor(out=sm[:, 3:4], in0=sm[:, 1:2], in1=sm[:, 2:3], op=ALU.mult), r=["sm"], w=["sm"])
                    A("dve", lambda e, s=s: e.tensor_scalar(out=comb[:, s, :], in0=lg[:], scalar1=m8[:, 0:1], scalar2=sm[:, 2:3],
                                                           op0=ALU.is_equal, op1=ALU.mult), r=["lg", "m8", "sm"], w=["comb"])
                    A("dve", lambda e: e.tensor_scalar(out=sm[:, 8:16], in0=lg[:], scalar1=m8[:, 1:2], scalar2=sm[:, 3:4],
                                                      op0=ALU.is_equal, op1=ALU.mult), r=["lg", "m8", "sm"], w=["sm"])
                    A("dve", lambda e, s=s: e.tensor_tensor(out=comb[:, s, :], in0=comb[:, s, :], in1=sm[:, 8:16], op=ALU.add),
                      r=["comb", "sm"], w=["comb"])
            return [f"hT{s}" for s in range(4)]

        HT = [f"hT{s}" for s in range(4)]
        KTN = lambda t: f"kT{t}"
        VN = lambda t: f"v{t}"

        for l in range(L):
            src = x if l == 0 else xmid
            dst = y if l == L - 1 else xmid
            dstn = "y" if l == L - 1 else "xmid"
            srcn = "x" if l == 0 else "xmid"
            A("sp", lambda e, l=l: e.dma_start(out=ppt[:], in_=pp[l * 128:(l + 1) * 128, :]), w=["ppt"], dma="ppt")
            A("sp", lambda e, l=l: e.dma_start(out=gqk_sb[:], in_=gqk[l * 128:(l + 1) * 128, :]), w=["gqk"], dma="gqk")
            for (bd, src_w, nm) in ((bd_r, rg_r, "bd_r"), (bd_i, rg_i, "bd_i")):
                A("dve", lambda e, bd=bd: e.memset(bd[:], 0.0), w=[nm])
                for h in range(8):
                    hp = (h % 2) * 64
                    r0 = (l * 8 + h) * 64
                    A("pool", lambda e, bd=bd, src_w=src_w, hp=hp, h=h, r0=r0: e.dma_start(
                        out=bd[hp:hp + 64, h // 2, hp:hp + 64], in_=src_w[r0:r0 + 64, :]), r=[nm], w=[nm], dma=nm)
            A("act", lambda e: e.activation(out=lc[:, 0:4], in_=pp2[:, PP_LAM:PP_LAM + 4], func=AF.Exp, scale=-1.0), r=["ppt"], w=["lc"])
            A("act", lambda e: e.activation(out=lc[:, 0:4], in_=lc[:, 0:4], func=AF.Ln, scale=1.0, bias=1.0), r=["lc"], w=["lc"])
            A("dve", lambda e: e.tensor_scalar(out=lc[:, 4:8], in0=lc[:, 0:4], scalar1=-LRU_C, scalar2=None, op0=ALU.mult), r=["lc"], w=["lc"])
            A("dve", lambda e: e.memset(histA[:], 0.0), w=["histA"])
            A("dve", lambda e: e.memset(histC[:], 0.0), w=["histC"])
            A("dve", lambda e: e.memset(hst[:], 0.0), w=["hst"])
            A("dve", lambda e: e.memset(gate_sb[:], -1e30), w=["gate_sb"])
            A("dve", lambda e: e.memset(bias_sb[:], 0.0), w=["bias_sb"])

            for c in range(NCH):
                t0 = c * 512
                stage[0] = 1

                A("sp", lambda e, t0=t0, src=src: e.dma_start(out=xt[:], in_=src[t0:t0 + 512, :].rearrange("(s p) d -> p s d", p=128)),
                  r=[f"{srcn}{c}"], w=["xt"], dma="xt")
                A("sp", lambda e, t0=t0: e.dma_start(out=cs[:], in_=rope[t0:t0 + 512, :].rearrange("(s p) d -> p s d", p=128)),
                  w=["cs"], dma="cs")
                rmsnorm(PP_GMIX, False)

                def wslab(j):
                    return load_slab(w_in[l * 1024:(l + 1) * 1024, j * 512:(j + 1) * 512].rearrange("(k p) c -> p k c", p=128), (8, 512))

                stage[0] = 2

                sxa, nxa = wslab(0)
                sba, nba = wslab(1)
                sca, nca = wslab(2)
                for i in range(4):
                    fs = slice(i * 128, (i + 1) * 128)
                    pxa, pxan = mm.get()
                    mm_group(pxa[:], [(sxa[:, k, fs], hT[:, k, :]) for k in range(8)], r=[nxa] + HT, w=[pxan])
                    pca, pcan = mm.get()
                    mm_group(pca[:], [(sca[:, k, fs], hT[:, k, :]) for k in range(8)], r=[nca] + HT, w=[pcan])
                    pba, pban = mm.get()
                    mm_group(pba[:], [(sba[:, k, fs], hT[:, k, :]) for k in range(8)], r=[nba] + HT, w=[pban])
                    xa_sb, xan = wk.get()
                    A("act", lambda e, xa_sb=xa_sb, pxa=pxa: e.copy(out=xa_sb[:], in_=pxa[:]), r=[pxan], w=[xan])
                    A("dve", lambda e, i=i: e.tensor_copy(out=ext[:, 0:2], in_=histA[:, i, :]), r=["histA"], w=["ext"])
                    A("dve", lambda e, xa_sb=xa_sb, pca=pca: e.tensor_tensor(out=ext[:, 2:514], in0=pca[:], in1=xa_sb[:], op=ALU.mult),
                      r=[pcan, xan, "ext"], w=["ext"])
                    A("dve", lambda e, i=i: e.tensor_copy(out=histA[:, i, :], in_=ext[:, 512:514]), r=["ext"], w=["histA"])
                    t1, t1n = wk.get()
                    cw = PP_CWA + i * 3
                    A("dve", lambda e, t1=t1, cw=cw: e.tensor_scalar(out=t1[:], in0=ext[:, 2:514], scalar1=col(cw + 2), scalar2=None, op0=ALU.mult),
                      r=["ext", "ppt"], w=[t1n])
                    A("dve", lambda e, t1=t1, cw=cw: e.scalar_tensor_tensor(out=t1[:], in0=ext[:, 1:513], scalar=col(cw + 1), in1=t1[:],
                                                                          op0=ALU.mult, op1=ALU.add), r=["ext", "ppt", t1n], w=[t1n])
                    A("dve", lambda e, t1=t1, cw=cw: e.scalar_tensor_tensor(out=t1[:], in0=ext[:, 0:512], scalar=col(cw), in1=t1[:],
                                                                          op0=ALU.mult, op1=ALU.add), r=["ext", "ppt", t1n], w=[t1n])
                    A("dve", lambda e, t1=t1, pba=pba, i=i: e.tensor_tensor(out=zT[0][:, i, :], in0=pba[:], in1=t1[:], op=ALU.mult),
                      r=[pban, t1n], w=[f"zA{i}"])

                stage[0] = 3

                def qk_proc(ps, psn, goff, dst_fn, dstn_fn, s):
                    sq, sqn = wk.get()
                    A("act", lambda e: e.activation(out=sq[:], in_=ps[:], func=AF.Square), r=[psn], w=[sqn])
                    A("dve", lambda e: e.tensor_reduce(out=sm[:, 16:24], in_=sq[:].rearrange("p (h d) -> p h d", h=8), axis=AX.X, op=ALU.add),
                      r=[sqn], w=["smq"])
                    A("act", lambda e: e.activation(out=sm[:, 16:24], in_=sm[:, 16:24], func=AF.Sqrt, scale=1.0 / 64, bias=EPS), r=["smq"], w=["smq"])
                    A("dve", lambda e: e.reciprocal(out=sm[:, 16:24], in_=sm[:, 16:24]), r=["smq"], w=["smq"])
                    qg, qgn = wk.get()
                    A("dve", lambda e: e.tensor_tensor(out=qg[:], in0=ps[:], in1=gqk_sb[:, goff:goff + 512], op=ALU.mult), r=[psn, "gqk"], w=[qgn])
                    q3 = qg[:].rearrange("p (h d) -> p h d", h=8)
                    rsb = sm[:, 16:24].rearrange("p (h o) -> p h o", o=1).to_broadcast([128, 8, 64])
                    A("dve", lambda e: e.tensor_tensor(out=q3, in0=q3, in1=rsb, op=ALU.mult), r=[qgn, "smq"], w=[qgn])
                    cosv = cs[:, s, 0:64].rearrange("p (h j) -> p h j", h=8)
                    sinv = cs[:, s, 64:128].rearrange("p (h j) -> p h j", h=8)
                    r1 = q3[:, :, 0:8]
                    r2 = q3[:, :, 8:16]
                    tt, ttn = wk.get()
                    tv = tt[:, 0:256].rearrange("p (a h j) -> p a h j", a=4, h=8)
                    A("dve", lambda e: e.tensor_tensor(out=tv[:, 0], in0=r1, in1=cosv, op=ALU.mult), r=[qgn, "cs"], w=[ttn])
                    A("dve", lambda e: e.tensor_tensor(out=tv[:, 1], in0=r2, in1=sinv, op=ALU.mult), r=[qgn, "cs", ttn], w=[ttn])
                    A("dve", lambda e: e.tensor_tensor(out=tv[:, 2], in0=r2, in1=cosv, op=ALU.mult), r=[qgn, "cs", ttn], w=[ttn])
                    A("dve", lambda e: e.tensor_tensor(out=tv[:, 3], in0=r1, in1=sinv, op=ALU.mult), r=[qgn, "cs", ttn], w=[ttn])
                    o3 = qn_bf[:].rearrange("p (h d) -> p h d", h=8)
                    A("dve", lambda e: e.tensor_tensor(out=o3[:, :, 0:8], in0=tv[:, 0], in1=tv[:, 1], op=ALU.subtract), r=[ttn], w=["qn_bf"])
                    A("dve", lambda e: e.tensor_tensor(out=o3[:, :, 8:16], in0=tv[:, 2], in1=tv[:, 3], op=ALU.add), r=[ttn, "qn_bf"], w=["qn_bf"])
                    A("act", lambda e: e.copy(out=o3[:, :, 16:64], in_=q3[:, :, 16:64]), r=[qgn, "qn_bf"], w=["qn_bf"])
                    tpb, tpn = tp.get()

                    def fn(e):
                        ins = None
                        for pr in range(4):
                            ins = e.transpose(out=tpb[:, pr * 128:(pr + 1) * 128], in_=qn_bf[:, pr * 128:(pr + 1) * 128], identity=id_bf[:])
                        return ins
                    A("pe", fn, r=["qn_bf", "id_bf"], w=[tpn])
                    tpv = tpb[:, 0:512].rearrange("p (a t) -> p a t", a=4)
                    if dst_fn is None:
                        qv = qT[:, :, s * 128:(s + 1) * 128].rearrange("p (a two) t -> p a two t", two=2)
                        A("act", lambda e: e.copy(out=qv[0:64, :, 0, :], in_=tpv[0:64]), r=[tpn], w=[dstn_fn])
                        A("act", lambda e: e.copy(out=qv[64:128, :, 1, :], in_=tpv[64:128]), r=[tpn, dstn_fn], w=[dstn_fn])
                    else:
                        A("act", lambda e: e.copy(out=dst_fn, in_=tpv), r=[tpn], w=[dstn_fn])

                sq_, nq_ = wslab(3)
                sk_, nk_ = wslab(4)
                sv_, nv_ = wslab(5)
                for s in range(4):
                    t = c * 4 + s
                    tsl = slice(s * 128, (s + 1) * 128)
                    pq, pqn = mm.get()
                    mm_group(pq[:], [(hT[:, k, tsl], sq_[:, k, :]) for k in range(8)], r=[nq_, f"hT{s}"], w=[pqn])
                    qk_proc(pq, pqn, 0, None, f"qT{s}", s)
                    pk, pkn = mm.get()
                    mm_group(pk[:], [(hT[:, k, tsl], sk_[:, k, :]) for k in range(8)], r=[nk_, f"hT{s}"], w=[pkn])
                    qk_proc(pk, pkn, 512, kT[:, :, t * 128:(t + 1) * 128], KTN(t), s)
                    pv, pvn = mm.get()
                    mm_group(pv[:], [(hT[:, k, tsl], sv_[:, k, :]) for k in range(8)], r=[nv_, f"hT{s}"], w=[pvn])
                    A("act", lambda e, pv=pv, t=t: e.copy(out=v_all[:, t, :], in_=pv[:]), r=[pvn], w=[VN(t)])
                for bb in range(2):
                    blk = c * 2 + bb
                    A("dve", lambda e, blk=blk: e.tensor_reduce(out=kmean_f[:, :, blk], in_=kT[:, :, blk * 256:(blk + 1) * 256], axis=AX.X, op=ALU.add),
                      r=[KTN(2 * blk), KTN(2 * blk + 1)], w=["kmean_f"])
                    A("act", lambda e, blk=blk: e.mul(out=kmean_bf[:, :, blk], in_=kmean_f[:, :, blk], mul=1.0 / 256), r=["kmean_f"], w=["kmean_bf"])

                stage[0] = 4

                sxc, nxc = wslab(6)
                sgc, ngc = wslab(7)
                for i in range(4):
                    fs = slice(i * 128, (i + 1) * 128)
                    pxc, pxcn = mm.get()
                    mm_group(pxc[:], [(sxc[:, k, fs], hT[:, k, :]) for k in range(8)], r=[nxc] + HT, w=[pxcn])
                    A("dve", lambda e, i=i: e.tensor_copy(out=ext[:, 0:3], in_=histC[:, i, :]), r=["histC"], w=["ext"])
                    A("act", lambda e, pxc=pxc: e.copy(out=ext[:, 3:515], in_=pxc[:]), r=[pxcn, "ext"], w=["ext"])
                    A("dve", lambda e, i=i: e.tensor_copy(out=histC[:, i, :], in_=ext[:, 512:515]), r=["ext"], w=["histC"])
                    xcc, xcn = wk.get()
                    cw = PP_CWC + i * 4
                    A("dve", lambda e, xcc=xcc, cw=cw, i=i: e.tensor_scalar(out=xcc[:], in0=ext[:, 3:515], scalar1=col(cw + 3), scalar2=col(PP_CBC + i),
                                                                          op0=ALU.mult, op1=ALU.add), r=["ext", "ppt"], w=[xcn])
                    for kk in range(3):
                        A("dve", lambda e, xcc=xcc, cw=cw, kk=kk: e.scalar_tensor_tensor(out=xcc[:], in0=ext[:, kk:kk + 512], scalar=col(cw + kk), in1=xcc[:],
                                                                                      op0=ALU.mult, op1=ALU.add), r=["ext", "ppt", xcn], w=[xcn])
                    xcb, xcbn = wkb.get()
                    A("act", lambda e, xcb=xcb, xcc=xcc: e.copy(out=xcb[:], in_=xcc[:]), r=[xcn], w=[xcbn])
                    pr_, prn = mm.get()
                    mm_group(pr_[:], [(bd_r[:, i, :], xcb[:])], r=["bd_r", xcbn], w=[prn])
                    pi_, pin = mm.get()
                    mm_group(pi_[:], [(bd_i[:, i, :], xcb[:])], r=["bd_i", xcbn], w=[pin])
                    rr, rrn = wk.get()
                    A("act", lambda e, rr=rr, pr_=pr_, i=i: e.activation(out=rr[:], in_=pr_[:], func=AF.Sigmoid, bias=col(PP_BR + i), scale=1.0), r=[prn, "ppt"], w=[rrn])
                    ig, ign = wk.get()
                    A("act", lambda e, ig=ig, pi_=pi_, i=i: e.activation(out=ig[:], in_=pi_[:], func=AF.Sigmoid, bias=col(PP_BI + i), scale=1.0), r=[pin, "ppt"], w=[ign])
                    A("dve", lambda e, rr=rr, i=i: e.tensor_scalar(out=rr[:], in0=rr[:], scalar1=lc[:, 4 + i:5 + i], scalar2=None, op0=ALU.mult), r=[rrn, "lc"], w=[rrn])
                    aa, aan = wk.get()
                    A("act", lambda e, aa=aa, rr=rr: e.activation(out=aa[:], in_=rr[:], func=AF.Exp), r=[rrn], w=[aan])
                    A("act", lambda e, rr=rr: e.activation(out=rr[:], in_=rr[:], func=AF.Exp, scale=2.0), r=[rrn], w=[rrn])
                    A("act", lambda e, rr=rr: e.activation(out=rr[:], in_=rr[:], func=AF.Sqrt, scale=-1.0, bias=1.0), r=[rrn], w=[rrn])
                    A("dve", lambda e, ig=ig, xcc=xcc: e.tensor_tensor(out=ig[:], in0=ig[:], in1=xcc[:], op=ALU.mult), r=[ign, xcn], w=[ign])
                    A("dve", lambda e, ig=ig, rr=rr: e.tensor_tensor(out=ig[:], in0=ig[:], in1=rr[:], op=ALU.mult), r=[ign, rrn], w=[ign])
                    A("dve", lambda e, xcc=xcc, aa=aa, ig=ig, i=i: e.tensor_tensor_scan(out=xcc[:], data0=aa[:], data1=ig[:], initial=hst[:, i:i + 1],
                                                                                     op0=ALU.mult, op1=ALU.add), r=[aan, ign, "hst", xcn], w=[xcn])
                    A("dve", lambda e, xcc=xcc, i=i: e.tensor_copy(out=hst[:, i:i + 1], in_=xcc[:, 511:512]), r=[xcn], w=["hst"])
                    pg, pgn = mm.get()
                    mm_group(pg[:], [(sgc[:, k, fs], hT[:, k, :]) for k in range(8)], r=[ngc] + HT, w=[pgn])
                    A("act", lambda e, aa=aa, pg=pg: e.activation(out=aa[:], in_=pg[:], func=AF.Gelu_apprx_tanh), r=[pgn, aan], w=[aan])
                    A("dve", lambda e, aa=aa, xcc=xcc, i=i: e.tensor_tensor(out=zT[2][:, i, :], in0=aa[:], in1=xcc[:], op=ALU.mult), r=[aan, xcn], w=[f"zC{i}"])

                stage[0] = 5
                for s in range(4):
                    t = c * 4 + s
                    b = t // 2
                    half = t % 2
                    tsl = slice(s * 128, (s + 1) * 128)
                    masked = b > MOBA_TOPK
                    if masked:
                        pgl, pgln = mm.get()

                        def fng(e, b=b, tsl=tsl, pgl=pgl):
                            ins = None
                            for h in range(8):
                                ins = e.matmul(pgl[:, h * 16:h * 16 + b], lhsT=qT[:, h, tsl], rhs=kmean_bf[:, h // 2, 0:b],
                                               start=True, stop=True)
                            return ins
                        A("pe", fng, r=[f"qT{s}", "kmean_bf"], w=[pgln])
                        A("dve", lambda e, b=b, pgl=pgl: e.tensor_copy(out=gate_sb[:, :, 0:b], in_=pgl[:, 0:128].rearrange("p (h n) -> p h n", h=8)[:, :, 0:b]),
                          r=[pgln], w=["gate_sb"])
                        for h in range(8):
                            A("dve", lambda e, h=h: e.max(out=m8[:, h * 8:(h + 1) * 8], in_=gate_sb[:, h, :]), r=["gate_sb"], w=["m8"])
                        for h in range(8):
                            A("dve", lambda e, h=h, b=b: e.tensor_scalar(out=bias_sb[:, h, 0:b], in0=gate_sb[:, h, 0:b], scalar1=m8[:, h * 8 + 2:h * 8 + 3],
                                                                        scalar2=NEGB, op0=ALU.is_lt, op1=ALU.mult), r=["gate_sb", "m8"], w=["bias_sb"])
                    units = []
                    for h in range(8):
                        hu = [("past", n, [2 * n, 2 * n + 1]) for n in range(max(0, b - int(os.environ.get("ATT_MAXPAST", 99))), b)]
                        hu.append(("own", b, [2 * b] if half == 0 else [2 * b, 2 * b + 1]))
                        for i, (kind, n, tiles) in enumerate(hu):
                            units.append((h, kind, n, tiles, i == 0, i == len(hu) - 1))

                    def emit_S(u):
                        h, kind, n, tiles, first, last = u
                        pair = h // 2
                        ps, psn = mm.get()
                        PT, PTn = PT_pool.get()
                        q_r = qT[:, h, tsl]
                        nt_ = len(tiles)
                        use_mask = masked and kind == "past"
                        r_s = [f"qT{s}"] + [KTN(kt) for kt in tiles]
                        B2 = None
                        if use_mask:
                            B2, B2n = B2_pool.get()
                            A("dve", lambda e: e.tensor_scalar(out=B2[:], in0=id_bf[:], scalar1=bias_sb[:, h, n:n + 1], scalar2=None, op0=ALU.mult),
                              r=["id_bf", "bias_sb"], w=[B2n])
                            r_s = r_s + [B2n, "ones_bf"]

                        def fn(e):
                            ins = None
                            for j, kt in enumerate(tiles):
                                ins = e.matmul(ps[:, j * 128:(j + 1) * 128], lhsT=kT[:, pair, kt * 128:(kt + 1) * 128], rhs=q_r,
                                               start=True, stop=not use_mask)
                                if use_mask:
                                    ins = e.matmul(ps[:, j * 128:(j + 1) * 128], lhsT=ones_bf[:], rhs=B2[:], start=False, stop=True)
                            return ins
                        A("pe", fn, r=r_s, w=[psn])
                        if kind == "past":
                            A("act", lambda e: e.activation(out=PT[:, 0:256], in_=ps[:, 0:256], func=AF.Exp, scale=0.125), r=[psn], w=[PTn])
                        else:
                            dj = nt_ - 1
                            if nt_ == 2:
                                A("act", lambda e: e.activation(out=PT[:, 0:128], in_=ps[:, 0:128], func=AF.Exp, scale=0.125), r=[psn], w=[PTn])
                            dt_, dtn = dtmp.get()
                            A("dve", lambda e: e.scalar_tensor_tensor(out=dt_[:], in0=ps[:, dj * 128:(dj + 1) * 128], scalar=0.125, in1=triT[:],
                                                                      op0=ALU.mult, op1=ALU.add), r=[psn, "triT"], w=[dtn])
                            A("act", lambda e: e.activation(out=PT[:, dj * 128:(dj + 1) * 128], in_=dt_[:], func=AF.Exp),
                              r=[dtn] + ([PTn] if nt_ == 2 else []), w=[PTn])
                        return (u, PT, PTn)

                    def emit_PV(item):
                        u, PT, PTn = item
                        h, kind, n, tiles, first, last = u
                        pair = h // 2
                        hp = (h % 2) * 64
                        nt_ = len(tiles)
                        k = pvc[0] % 2
                        pvc[0] += 1
                        po, pd = psOut[k], psDen[k]
                        pon, pdn = f"pso{k}", f"psd{k}"

                        def fnV(e):
                            ins = None
                            for j, kt in enumerate(tiles):
                                e.matmul(po[:, 0:128], lhsT=v_all[:, kt, pair * 128:(pair + 1) * 128], rhs=PT[:, j * 128:(j + 1) * 128],
                                         start=(j == 0), stop=(j == nt_ - 1))
                                ins = e.matmul(pd[:, 0:128], lhsT=ones_bf[:], rhs=PT[:, j * 128:(j + 1) * 128], start=(j == 0), stop=(j == nt_ - 1))
                            return ins
                        A("pe", fnV, r=[PTn, "ones_bf"] + [VN(kt) for kt in tiles], w=[pon, pdn])
                        ao, ad = accO[h % 2], accD[h % 2]
                        aon, adn = f"accO{h % 2}", f"accD{h % 2}"
                        if first:
                            A("dve", lambda e: e.tensor_copy(out=ao[hp:hp + 64, :], in_=po[hp:hp + 64, 0:128]), r=[pon], w=[aon])
                            A("dve", lambda e: e.tensor_copy(out=ad[hp:hp + 64, :], in_=pd[hp:hp + 64, 0:128]), r=[pdn], w=[adn])
                        else:
                            A("dve", lambda e: e.tensor_tensor(out=ao[hp:hp + 64, :], in0=po[hp:hp + 64, 0:128], in1=ao[hp:hp + 64, :], op=ALU.add),
                              r=[pon, aon], w=[aon])
                            A("dve", lambda e: e.tensor_tensor(out=ad[hp:hp + 64, :], in0=pd[hp:hp + 64, 0:128], in1=ad[hp:hp + 64, :], op=ALU.add),
                              r=[pdn, adn], w=[adn])
                        if last:
                            zdst = zT[1][hp:hp + 64, pair, tsl]
                            A("dve", lambda e: e.reciprocal(out=ad[hp:hp + 64, :], in_=ad[hp:hp + 64, :]), r=[adn], w=[adn])
                            A("dve", lambda e: e.tensor_tensor(out=zdst, in0=ao[hp:hp + 64, :], in1=ad[hp:hp + 64, :], op=ALU.mult),
                              r=[aon, adn], w=[f"zB{s}"])

                    prev = None
                    for u in units:
                        item = emit_S(u)
                        if prev is not None:
                            emit_PV(prev)
                        prev = item
                    emit_PV(prev)
                stage[0] = 5.8
                if dbg:
                    for (zi, nm, rd) in ((0, "d_za", [f"zA{i}" for i in range(4)]), (1, "d_zb", [f"zB{i}" for i in range(4)]), (2, "d_zc", [f"zC{i}" for i in range(4)])):
                        if l == 0:
                            A("sp", lambda e, zi=zi, nm=nm, t0=t0: e.dma_start(out=dbg_out[nm][:, t0:t0 + 512].rearrange("(a p) t -> p a t", p=128), in_=zT[zi][:]),
                              r=rd, w=[nm], dma="dbg" + nm)

                stage[0] = 6

                ZN = [[f"zA{i}" for i in range(4)], [f"zB{i}" for i in range(4)], [f"zC{i}" for i in range(4)]]
                for mq in range(2):
                    for br in range(3):
                        wsl, wn = load_slab(wo_abc[br][l * 512:(l + 1) * 512, mq * 512:(mq + 1) * 512].rearrange("(k p) c -> p k c", p=128), (4, 512))
                        gsl, gn = wslab(8 + br * 2 + mq)
                        for mi in range(4):
                            m = mq * 4 + mi
                            fs = slice(mi * 128, (mi + 1) * 128)
                            py, pyn = mm.get()
                            mm_group(py[:], [(wsl[:, k, fs], zT[br][:, k, :]) for k in range(4)], r=[wn] + ZN[br], w=[pyn])
                            pgt, pgtn = mm.get()
                            mm_group(pgt[:], [(gsl[:, k, fs], hT[:, k, :]) for k in range(8)], r=[gn] + HT, w=[pgtn])
                            sg, sgn = wk.get()
                            A("act", lambda e, sg=sg, pgt=pgt: e.activation(out=sg[:], in_=pgt[:], func=AF.Sigmoid), r=[pgtn], w=[sgn])
                            if br == 0:
                                A("dve", lambda e, sg=sg, py=py, mi=mi: e.tensor_tensor(out=macc[:, mi, :], in0=py[:], in1=sg[:], op=ALU.mult), r=[pyn, sgn], w=[f"macc{mi}"])
                            else:
                                A("dve", lambda e, sg=sg, py=py: e.tensor_tensor(out=sg[:], in0=py[:], in1=sg[:], op=ALU.mult), r=[pyn, sgn], w=[sgn])
                                if br == 1:
                                    A("dve", lambda e, sg=sg, mi=mi: e.tensor_tensor(out=macc[:, mi, :], in0=macc[:, mi, :], in1=sg[:], op=ALU.add), r=[sgn, f"macc{mi}"], w=[f"macc{mi}"])
                                else:
                                    A("dve", lambda e, sg=sg, mi=mi, m=m: e.tensor_tensor(out=mergedT[:, m, :], in0=macc[:, mi, :], in1=sg[:], op=ALU.add), r=[sgn, f"macc{mi}"], w=[f"mg{m}"])
                MG = [f"mg{m}" for m in range(8)]
                if dbg and l == 0:
                    A("sp", lambda e, t0=t0: e.dma_start(out=dbg_out["d_mg"][:, t0:t0 + 512].rearrange("(a p) t -> p a t", p=128), in_=mergedT[:]),
                      r=MG, w=["d_mg"], dma="dbgmg")
                for hf in range(2):
                    osl, on = load_slab(w_o[l * 1024:(l + 1) * 1024, hf * 512:(hf + 1) * 512].rearrange("(k p) c -> p k c", p=128), (8, 512))
                    for s in range(4):
                        tsl = slice(s * 128, (s + 1) * 128)
                        po, pon = mm.get()
                        mm_group(po[:], [(mergedT[:, k, tsl], osl[:, k, :]) for k in range(8)], r=[on] + MG, w=[pon])
                        A("dve", lambda e, po=po, s=s, hf=hf: e.tensor_tensor(out=xt[:, s, hf * 512:(hf + 1) * 512], in0=po[:], in1=xt[:, s, hf * 512:(hf + 1) * 512], op=ALU.add),
                          r=[pon, "xt"], w=["xt"])
                if dbg and l == 0:
                    A("sp", lambda e, t0=t0: e.dma_start(out=dbg_out["d_xmix"][t0:t0 + 512, :].rearrange("(s p) d -> p s d", p=128), in_=xt[:]),
                      r=["xt"], w=["d_xmix"], dma="dbgx")

                stage[0] = 7

                is_moe = (l % 2 == 1)
                rmsnorm(PP_GFFN, is_moe)
                if not is_moe:
                    groups = [(None, f0, min(4, 22 - f0)) for f0 in range(0, 22, 4)]
                else:
                    groups = [(ex, f0, 4) for ex in range(8) for f0 in range(0, 28, 4)]
                for (ex, f0, nf) in groups:
                    if ex is None:
                        gsrc = ffn_g[:, f0 * 128:(f0 + nf) * 128]
                        usrc = ffn_u[:, f0 * 128:(f0 + nf) * 128]
                        dsrc = ffn_d[f0 * 128:(f0 + nf) * 128, :]
                    else:
                        gsrc = moe_g[ex * 1024:(ex + 1) * 1024, f0 * 128:(f0 + nf) * 128]
                        usrc = moe_u[ex * 1024:(ex + 1) * 1024, f0 * 128:(f0 + nf) * 128]
                        dsrc = moe_d[ex * 3584 + f0 * 128:ex * 3584 + (f0 + nf) * 128, :]
                    gsl, gn = load_slab(gsrc.rearrange("(k p) c -> p k c", p=128), (8, nf * 128))
                    usl, un = load_slab(usrc.rearrange("(k p) c -> p k c", p=128), (8, nf * 128))
                    dsl, dn = load_slab(dsrc.rearrange("(f p) c -> p f c", p=128), (nf, 1024))
                    aT, aTn = aT_pool.get()
                    for f in range(nf):
                        fs = slice(f * 128, (f + 1) * 128)
                        pgg, pggn = mm.get()
                        mm_group(pgg[:], [(gsl[:, k, fs], hT[:, k, :]) for k in range(8)], r=[gn] + HT, w=[pggn])
                        puu, puun = mm.get()
                        mm_group(puu[:], [(usl[:, k, fs], hT[:, k, :]) for k in range(8)], r=[un] + HT, w=[puun])
                        sg, sgn = wk.get()
                        A("act", lambda e, sg=sg, pgg=pgg: e.activation(out=sg[:], in_=pgg[:], func=AF.Silu), r=[pggn], w=[sgn])
                        A("dve", lambda e, sg=sg, puu=puu, aT=aT, f=f: e.tensor_tensor(out=aT[:, f, :], in0=puu[:], in1=sg[:], op=ALU.mult), r=[puun, sgn], w=[f"{aTn}_{f}"])
                    aN = [f"{aTn}_{f}" for f in range(nf)]
                    for s in range(4):
                        tsl = slice(s * 128, (s + 1) * 128)
                        for hf in range(2):
                            pd, pdn = mm.get()
                            mm_group(pd[:], [(aT[:, f, tsl], dsl[:, f, hf * 512:(hf + 1) * 512]) for f in range(nf)], r=[dn] + aN, w=[pdn])
                            xv = xt[:, s, hf * 512:(hf + 1) * 512]
                            if ex is None:
                                A("dve", lambda e, pd=pd, xv=xv: e.tensor_tensor(out=xv, in0=pd[:], in1=xv, op=ALU.add), r=[pdn, "xt"], w=["xt"])
                            else:
                                A("dve", lambda e, pd=pd, xv=xv, s=s, ex=ex: e.scalar_tensor_tensor(out=xv, in0=pd[:], scalar=comb[:, s, ex:ex + 1], in1=xv,
                                                                                                 op0=ALU.mult, op1=ALU.add), r=[pdn, "xt", "comb"], w=["xt"])
                stage[0] = 0
                A("sp", lambda e, t0=t0, dst=dst: e.dma_start(out=dst[t0:t0 + 512, :].rearrange("(s p) d -> p s d", p=128), in_=xt[:]),
                  r=["xt"], w=[f"{dstn}{c}"], dma="xt_st")
        fin = [f"y{c}" for c in range(NCH)] + (list(dbg_out.keys()) if dbg else [])
        A("sp", lambda e: e.nop(), r=fin)
        Sc.emit(st)
        nc._n_ops = len(Sc.ops)
    return nc


def host_prep(inputs, S=4096):
    f = lambda k: np.asarray(inputs[k], dtype=np.float32)
    L = 2
    pp = np.zeros((L, 128, 64), np.float32)
    for l in range(L):
        pp[l, :, PP_GMIX:PP_GMIX + 8] = f("norm_mix")[l].reshape(8, 128).T
        pp[l, :, PP_GFFN:PP_GFFN + 8] = f("norm_ffn")[l].reshape(8, 128).T
        ca = f("conv_a_w")[l]
        pp[l, :, PP_CWA:PP_CWA + 12] = ca.reshape(3, 4, 128).transpose(2, 1, 0).reshape(128, 12)
        cc = f("conv_c_w")[l]
        pp[l, :, PP_CWC:PP_CWC + 16] = cc.reshape(4, 4, 128).transpose(2, 1, 0).reshape(128, 16)
        pp[l, :, PP_CBC:PP_CBC + 4] = f("conv_c_b")[l].reshape(4, 128).T
        pp[l, :, PP_BR:PP_BR + 4] = f("rg_b_r")[l].reshape(4, 128).T
        pp[l, :, PP_BI:PP_BI + 4] = f("rg_b_i")[l].reshape(4, 128).T
        pp[l, :, PP_LAM:PP_LAM + 4] = f("rg_lambda")[l].reshape(4, 128).T
    gqk = np.zeros((L, 128, 1024), np.float32)
    for l in range(L):
        gqk[l, :, 0:512] = np.tile(f("q_norm")[l], 8)[None, :]
        gqk[l, :, 512:1024] = np.tile(f("k_norm")[l], 8)[None, :]
    half = ROT_DIM // 2
    inv_freq = (ROPE_THETA ** (-np.arange(0, ROT_DIM, 2, dtype=np.float32) / ROT_DIM)).astype(np.float32)
    ang = np.arange(S, dtype=np.float32)[:, None] * inv_freq[None, :]
    rope = np.concatenate([np.tile(np.cos(ang), (1, 8)), np.tile(np.sin(ang), (1, 8))], axis=1).astype(np.float32)
    shared = {
        "w_in": f("w_in").reshape(2 * 1024, 7168),
        "wo_a": f("wo_a").reshape(2 * 512, 1024), "wo_b": f("wo_b").reshape(2 * 512, 1024), "wo_c": f("wo_c").reshape(2 * 512, 1024),
        "w_o": f("w_o").reshape(2 * 1024, 1024),
        "ffn_w_gate": f("ffn_w_gate").reshape(1024, 2816), "ffn_w_up": f("ffn_w_up").reshape(1024, 2816),
        "ffn_w_down": f("ffn_w_down").reshape(2816, 1024),
        "moe_router": f("moe_router").reshape(1024, 8),
        "moe_w_gate": f("moe_w_gate").reshape(8 * 1024, 3584), "moe_w_up": f("moe_w_up").reshape(8 * 1024, 3584),
        "moe_w_down": f("moe_w_down").reshape(8 * 3584, 1024),
        "rg_w_r": f("rg_w_r").reshape(2 * 8 * 64, 64), "rg_w_i": f("rg_w_i").reshape(2 * 8 * 64, 64),
        "pp": pp.reshape(2 * 128, 64), "gqk": gqk.reshape(2 * 128, 1024), "rope": rope,
    }
    return shared


_NC_CACHE = {}


def kernel(**inputs):
    x = np.asarray(inputs["x"], dtype=np.float32)
    B, S, D = x.shape
    shared = host_prep(inputs, S)
    if S not in _NC_CACHE:
        _NC_CACHE[S] = build_nc(S=S)
    nc = _NC_CACHE[S]
    in_maps = [dict(shared, x=np.ascontiguousarray(x[b])) for b in range(B)]
    res = run_bass_kernel_spmd(nc, in_maps, core_ids=list(range(B)))
    return np.stack([np.asarray(r["y"], dtype=np.float32) for r in res.results], axis=0)
```
